# Optimizing a Trainium2 kernel written in Bass

```python
import jax, jax.numpy as jnp
from jax import lax
import numpy as np

D_MODEL = 4096
BATCH = 2
SEQ = 8192
DEPTH = 2

CHUNK = 64
EPS = 1e-6
HG_DK = 128
HG_HEADS = D_MODEL // (2 * HG_DK)
HG_DV = 128
HG_WIDTH = HG_HEADS * HG_DV
HG_FDIM = HG_HEADS * HG_DK
MLA_DV = 128
MLA_HEADS = D_MODEL // (2 * MLA_DV)
MLA_NOPE = 128
MLA_ROPE = 64
MLA_Q_RANK = 768
MLA_KV_RANK = 512
MLA_WIDTH = MLA_HEADS * MLA_DV
ROPE_THETA = 10000.0
Q_BLOCK = 128
IN_SIZES = (HG_FDIM, HG_FDIM, HG_WIDTH, HG_WIDTH, MLA_Q_RANK, MLA_KV_RANK, MLA_ROPE, D_MODEL, D_MODEL)
IN_COLS = 2 * HG_FDIM + 2 * HG_WIDTH + MLA_Q_RANK + MLA_KV_RANK + MLA_ROPE + 2 * D_MODEL
D_FF = ((8 * D_MODEL // 3 + 255) // 256) * 256
N_EXPERTS = 8
TOP_K = 2
D_FF_EXPERT = D_MODEL
N_DENSE = (DEPTH + 1) // 2
N_MOE = DEPTH // 2

kernel_name = "hgrn2_mla_gated_hybrid_moe"


def rmsnorm(x, g):
    xf = x.astype(jnp.float32)
    y = xf * lax.rsqrt(jnp.mean(xf * xf, axis=-1, keepdims=True) + EPS)
    return (y * g.astype(jnp.float32)).astype(x.dtype)


def apply_rope(x, cos, sin):
    half = x.shape[-1] // 2
    xf = x.astype(jnp.float32)
    x1, x2 = xf[..., :half], xf[..., half:]
    return jnp.concatenate([x1 * cos - x2 * sin, x2 * cos + x1 * sin], axis=-1).astype(x.dtype)


def hgrn2_branch(q_raw, f_raw, i_raw, og_raw, lb, gain):
    B, S, _ = q_raw.shape
    NC = S // CHUNK
    z = f_raw.astype(jnp.float32)
    log_f = jnp.logaddexp(jnp.log(lb), jnp.log1p(-lb) + jax.nn.log_sigmoid(z))
    k = -jnp.expm1(log_f)
    q = jax.nn.silu(q_raw.astype(jnp.float32))
    v = i_raw.astype(jnp.float32)

    def to_chunks(t, d):
        return t.reshape(B, NC, CHUNK, HG_HEADS, d).transpose(1, 0, 3, 2, 4)

    causal = jnp.tril(jnp.ones((CHUNK, CHUNK), dtype=bool))

    def step(state, xs):
        qc, kc, vc, gc = xs
        b = jnp.cumsum(gc, axis=2)
        b_last = b[:, :, -1, :]
        o_inter = jnp.einsum('bhtc,bhcv->bhtv', qc * jnp.exp(b), state)
        diff = b[:, :, :, None, :] - b[:, :, None, :, :]
        decay = jnp.where(causal[:, :, None], jnp.exp(jnp.minimum(diff, 0.0)), 0.0)
        scores = jnp.einsum('bhtc,bhsc,bhtsc->bhts', qc, kc, decay)
        o_intra = jnp.einsum('bhts,bhsv->bhtv', scores, vc)
        k_dec = kc * jnp.exp(b_last[:, :, None, :] - b)
        state = jnp.exp(b_last)[..., None] * state + jnp.einsum('bhsc,bhsv->bhcv', k_dec, vc)
        return state, o_intra + o_inter

    state0 = jnp.zeros((B, HG_HEADS, HG_DK, HG_DV), jnp.float32)
    _, o = lax.scan(step, state0, (to_chunks(q, HG_DK), to_chunks(k, HG_DK), to_chunks(v, HG_DV), to_chunks(log_f, HG_DK)))
    o = o.transpose(1, 0, 3, 2, 4).reshape(B, S, HG_HEADS, HG_DV)
    o = o * lax.rsqrt(jnp.mean(o * o, axis=-1, keepdims=True) + EPS)
    o = o.reshape(B, S, HG_WIDTH) * gain.astype(jnp.float32) * jax.nn.silu(og_raw.astype(jnp.float32))
    return o.astype(q_raw.dtype)


def mla_branch(c_q, c_kv, k_pe_raw, q_gain, kv_gain, w_uq, w_ukv, cos, sin):
    B, S, _ = c_q.shape
    q = (rmsnorm(c_q, q_gain) @ w_uq).reshape(B, S, MLA_HEADS, MLA_NOPE + MLA_ROPE)
    q_nope = q[..., :MLA_NOPE]
    q_pe = apply_rope(q[..., MLA_NOPE:], cos[:, :, None, :], sin[:, :, None, :])
    kv = (rmsnorm(c_kv, kv_gain) @ w_ukv).reshape(B, S, MLA_HEADS, MLA_NOPE + MLA_DV)
    k_nope, v = kv[..., :MLA_NOPE], kv[..., MLA_NOPE:]
    k_pe = apply_rope(k_pe_raw, cos, sin)
    scale = (MLA_NOPE + MLA_ROPE) ** -0.5
    key_chunk = jnp.arange(S) // CHUNK

    def block(j):
        start = j * Q_BLOCK
        qn = lax.dynamic_slice_in_dim(q_nope, start, Q_BLOCK, axis=1)
        qp = lax.dynamic_slice_in_dim(q_pe, start, Q_BLOCK, axis=1)
        s = (jnp.einsum('bqhd,bkhd->bhqk', qn, k_nope) + jnp.einsum('bqhr,bkr->bhqk', qp, k_pe)).astype(jnp.float32) * scale
        q_chunk = (start + jnp.arange(Q_BLOCK)) // CHUNK
        mask = key_chunk[None, :] <= q_chunk[:, None]
        p = jax.nn.softmax(jnp.where(mask, s, -jnp.inf), axis=-1).astype(v.dtype)
        return jnp.einsum('bhqk,bkhd->bqhd', p, v)

    o = lax.map(block, jnp.arange(S // Q_BLOCK))
    return o.transpose(1, 0, 2, 3, 4).reshape(B, S, MLA_WIDTH)


def hybrid_mixer(h, w_in, lb, hg_gain, q_gain, kv_gain, w_uq, w_ukv, w_branch, w_out, cos, sin):
    proj = h @ w_in
    points = np.cumsum(np.array(IN_SIZES))[:-1].tolist()
    hq, hf, hi, hog, cq, ckv, kpe, ga, gb = jnp.split(proj, points, axis=-1)
    o_a = hgrn2_branch(hq, hf, hi, hog, lb, hg_gain)
    o_b = mla_branch(cq, ckv, kpe, q_gain, kv_gain, w_uq, w_ukv, cos, sin)
    y_a = o_a @ w_branch[:HG_WIDTH]
    y_b = o_b @ w_branch[HG_WIDTH:]
    y = jax.nn.sigmoid(ga) * y_a + jax.nn.sigmoid(gb) * y_b
    return y @ w_out


def swiglu(h, w1, w3, w2):
    return (jax.nn.silu(h @ w1) * (h @ w3)) @ w2


def moe_ffn(h, w_router, w1, w3, w2):
    B, S, D = h.shape
    t = h.reshape(B * S, D)
    logits = (t @ w_router).astype(jnp.float32)
    top_v, top_i = lax.top_k(logits, TOP_K)
    top_w = jax.nn.softmax(top_v, axis=-1)
    combine = jnp.sum(jax.nn.one_hot(top_i, N_EXPERTS, dtype=jnp.float32) * top_w[..., None], axis=1)
    out = jnp.zeros_like(t)
    for e in range(N_EXPERTS):
        out = out + combine[:, e:e + 1].astype(t.dtype) * swiglu(t, w1[e], w3[e], w2[e])
    return out.reshape(B, S, D)


def setup_inputs(seed: int = 0) -> dict:
    key = jax.random.key(seed)
    ks = jax.random.split(key, 24)

    def nrm(k, shape, fan_in):
        return jax.random.normal(k, shape, jnp.float32) * fan_in ** -0.5

    def gain(k, shape):
        return 1.0 + 0.02 * jax.random.normal(k, shape, jnp.float32)

    x = jax.random.normal(ks[0], (BATCH, SEQ, D_MODEL), jnp.float32)
    offsets = jax.random.randint(ks[1], (BATCH, 1), 0, 4096, dtype=jnp.int32)
    positions = offsets + jnp.arange(SEQ, dtype=jnp.int32)[None, :]
    return {
        "x": x,
        "positions": positions,
        "norm_mix": gain(ks[2], (DEPTH, D_MODEL)),
        "w_in": nrm(ks[3], (DEPTH, D_MODEL, IN_COLS), D_MODEL),
        "hg_lb_logits": 0.5 * jax.random.normal(ks[4], (DEPTH, HG_FDIM), jnp.float32),
        "hg_norm": gain(ks[5], (DEPTH, HG_WIDTH)),
        "mla_q_norm": gain(ks[6], (DEPTH, MLA_Q_RANK)),
        "w_uq": nrm(ks[7], (DEPTH, MLA_Q_RANK, MLA_HEADS * (MLA_NOPE + MLA_ROPE)), MLA_Q_RANK),
        "mla_kv_norm": gain(ks[8], (DEPTH, MLA_KV_RANK)),
        "w_ukv": nrm(ks[9], (DEPTH, MLA_KV_RANK, MLA_HEADS * (MLA_NOPE + MLA_DV)), MLA_KV_RANK),
        "w_branch": nrm(ks[10], (DEPTH, HG_WIDTH + MLA_WIDTH, D_MODEL), HG_WIDTH),
        "w_out": nrm(ks[11], (DEPTH, D_MODEL, D_MODEL), D_MODEL),
        "norm_ffn": gain(ks[12], (DEPTH, D_MODEL)),
        "ffn_w1": nrm(ks[13], (N_DENSE, D_MODEL, D_FF), D_MODEL),
        "ffn_w3": nrm(ks[14], (N_DENSE, D_MODEL, D_FF), D_MODEL),
        "ffn_w2": nrm(ks[15], (N_DENSE, D_FF, D_MODEL), D_FF),
        "w_router": nrm(ks[16], (N_MOE, D_MODEL, N_EXPERTS), D_MODEL),
        "moe_w1": nrm(ks[17], (N_MOE, N_EXPERTS, D_MODEL, D_FF_EXPERT), D_MODEL),
        "moe_w3": nrm(ks[18], (N_MOE, N_EXPERTS, D_MODEL, D_FF_EXPERT), D_MODEL),
        "moe_w2": nrm(ks[19], (N_MOE, N_EXPERTS, D_FF_EXPERT, D_MODEL), D_FF_EXPERT),
        "norm_final": gain(ks[20], (D_MODEL,)),
    }


def reference(x, positions, norm_mix, w_in, hg_lb_logits, hg_norm, mla_q_norm, w_uq, mla_kv_norm, w_ukv,
              w_branch, w_out, norm_ffn, ffn_w1, ffn_w3, ffn_w2, w_router, moe_w1, moe_w3, moe_w2, norm_final):
    half = MLA_ROPE // 2
    inv_freq = ROPE_THETA ** (-jnp.arange(half, dtype=jnp.float32) / half)
    ang = positions.astype(jnp.float32)[..., None] * inv_freq
    cos, sin = jnp.cos(ang), jnp.sin(ang)
    lbs = jnp.cumsum(jax.nn.softmax(hg_lb_logits.astype(jnp.float32), axis=0), axis=0)
    lbs = lbs - lbs[0:1]
    for l in range(DEPTH):
        h = rmsnorm(x, norm_mix[l])
        x = x + hybrid_mixer(h, w_in[l], lbs[l], hg_norm[l], mla_q_norm[l], mla_kv_norm[l], w_uq[l], w_ukv[l],
                             w_branch[l], w_out[l], cos, sin)
        h = rmsnorm(x, norm_ffn[l])
        if l % 2 == 0:
            x = x + swiglu(h, ffn_w1[l // 2], ffn_w3[l // 2], ffn_w2[l // 2])
        else:
            x = x + moe_ffn(h, w_router[l // 2], moe_w1[l // 2], moe_w3[l // 2], moe_w2[l // 2])
    return rmsnorm(x, norm_final)
```

```python
import numpy as np
from contextlib import ExitStack
import concourse.bass as bass
import concourse.mybir as mybir
from concourse.bass_utils import run_bass_kernel_spmd

F32 = mybir.dt.float32
BF16 = mybir.dt.bfloat16
I32 = mybir.dt.int32
AF = mybir.ActivationFunctionType
ALU = mybir.AluOpType
AX = mybir.AxisListType

NCORES = 2


class Sched:
    SEM_ROT = 30000
    N_DMA_SEMS = 6

    def __init__(self, nc, es):
        self.nc = nc
        self.es = es
        self.ops = []
        self.engs = {"pe": nc.tensor, "act": nc.scalar, "dve": nc.vector,
                     "pool": nc.gpsimd, "sp": nc.sync}
        self.comp_cnt = {}
        self.dma_cnt = {}
        self.sem_objs = {}
        self.clock = {e: {} for e in self.engs}
        self.tot_ops = 0
        self.tot_waits = 0

    def op(self, eng, fn, r=(), w=(), dma=False, alldma=False):
        self.ops.append((eng, fn, tuple(r), tuple(w), dma, alldma))

    def _newsem(self, name):
        return self.es.enter_context(self.nc.semaphore(name))

    def _sem(self, key):
        s = self.sem_objs.get(key)
        if s is None:
            s = self._newsem("_".join(str(k) for k in key))
            self.sem_objs[key] = s
        return s

    def flush(self):
        ops = self.ops
        n = len(ops)
        last_w = {}
        readers = {}
        deps = [None] * n
        signal = [False] * n
        for i, (eng, fn, r, w, dma, alldma) in enumerate(ops):
            per_eng = {}
            dma_deps = set()
            rset = set(r)

            def add(j, raw):
                oj = ops[j]
                if oj[4]:
                    dma_deps.add(j)
                    return
                if oj[0] == eng and not dma and not raw:
                    return
                if per_eng.get(oj[0], -1) < j:
                    per_eng[oj[0]] = j

            for x in r:
                j = last_w.get(x)
                if j is not None:
                    add(j, True)
            for x in w:
                j = last_w.get(x)
                if j is not None:
                    add(j, x in rset)
                rd = readers.get(x)
                if rd:
                    for e2, j in rd.items():
                        if e2 is None:
                            for jj in j:
                                add(jj, False)
                        else:
                            add(j, False)
            keep = list(per_eng.values()) + list(dma_deps)
            for j in keep:
                signal[j] = True
            deps[i] = keep
            for x in w:
                last_w[x] = i
                readers[x] = {}
            for x in r:
                rd = readers.setdefault(x, {})
                if dma:
                    rd.setdefault(None, []).append(i)
                else:
                    rd[eng] = i

        sig = [None] * n
        guard = [None] * n
        dma_pos = [None] * n
        for i, (eng, fn, r, w, dma, alldma) in enumerate(ops):
            if dma:
                k = self.dma_cnt.get(eng, 0)
                self.dma_cnt[eng] = k + 1
                slot = k % self.N_DMA_SEMS
                key = ("d", eng, slot)
                idx = k // self.N_DMA_SEMS + 1
                sig[i] = (key, 16 * idx)
                if idx > 1:
                    guard[i] = (key, 16 * (idx - 1))
                dma_pos[i] = k + 1
            elif signal[i]:
                k = self.comp_cnt.get(eng, 0)
                self.comp_cnt[eng] = k + 1
                gen = k // self.SEM_ROT
                sig[i] = (("c", eng, gen), k % self.SEM_ROT + 1)

        kc = [None] * n
        issued = dict()
        base_cnt = {e: self.dma_cnt.get(e, 0) - sum(1 for o in ops if o[4] and o[0] == e) for e in self.dma_cnt}
        for e, v in base_cnt.items():
            issued[e] = v
        nwaits = 0
        for i, (eng, fn, r, w, dma, alldma) in enumerate(ops):
            ck = self.clock[eng]
            E = self.engs[eng]
            need = []
            if guard[i] is not None:
                need.append((guard[i], None))
            for j in deps[i]:
                need.append((sig[j], j))
            if alldma:
                for e2, cnt_ in issued.items():
                    for slot in range(min(self.N_DMA_SEMS, cnt_)):
                        nd = (cnt_ - slot + self.N_DMA_SEMS - 1) // self.N_DMA_SEMS
                        need.append(((("d", e2, slot), 16 * nd), None))
            for (key, val), j in need:
                if ck.get(key, 0) >= val:
                    continue
                E.wait_ge(self._sem(key), val)
                nwaits += 1
                ck[key] = val
                if j is not None and kc[j] is not None:
                    for k2, v2 in kc[j].items():
                        if ck.get(k2, 0) < v2:
                            ck[k2] = v2
            inst = fn()
            if dma:
                issued[eng] = dma_pos[i]
            if sig[i] is not None:
                key, val = sig[i]
                inst.then_inc(self._sem(key), 16 if dma else 1)
                snap = dict(ck)
                if snap.get(key, 0) < val:
                    snap[key] = val
                kc[i] = snap
        self.tot_ops += n
        self.tot_waits += nwaits
        self.ops = []

    def stats(self):
        return dict(n_ops=self.tot_ops, n_waits=self.tot_waits, n_sems=len(self.sem_objs))


D = 4096
B_, S_ = 2, 8192
SB = S_
TT = 1024
NT5 = 512
HGW = 2048
NOPE, ROPE, MLA_DV = 128, 64, 128
QR, KVR = 768, 512
IN_COLS = 17728
DFF = 11008
NEXP = 8
EPS = 1e-6
CHUNK = 64
HPC = 4
NHG = 16 // HPC
ATT_SCALE = float((NOPE + ROPE) ** -0.5)
R_HQ, R_HF, R_HI, R_HOG, R_CQ, R_CKV, R_KPE, R_GA, R_GB = 0, 2048, 4096, 6144, 8192, 8960, 9472, 9536, 13632
R_K = IN_COLS
A_ROWS = IN_COLS + 2048


class Ctx:
    def __init__(self):
        self.nc = bass.Bass("TRN2", target_bir_lowering=False)
        self.es = ExitStack()
        self.S = Sched(self.nc, self.es)
        nc = self.nc
        self.ps = [self.es.enter_context(nc.psum_tensor(f"ps{i}", [128, NT5], F32)) for i in range(8)]
        self.ps_i = 0
        self.bar = self.es.enter_context(nc.sbuf_tensor("bar_scr", [128, 8], F32))
        self.pes = ExitStack()
        self.wb = []
        self.wb_i = 0
        self.wcols = 512
        self.nphase = 0
        self.nbar = 0

    def sb(self, name, shape, dt):
        return self.pes.enter_context(self.nc.sbuf_tensor(f"p{self.nphase}_{name}", shape, dt))

    def alloc_wb(self, n, wcols=512):
        self.wcols = wcols
        self.wb = [self.sb(f"wb{i}", [128, 32, wcols], BF16) for i in range(n)]
        self.wb_i = 0

    def next_ps(self):
        i = self.ps_i
        self.ps_i = (i + 1) % 8
        return i

    def next_wb(self):
        i = self.wb_i
        self.wb_i = (i + 1) % len(self.wb)
        return i

    def barrier(self):
        nc, S, bar = self.nc, self.S, self.bar
        k = self.nbar
        self.nbar += 1
        engs = ("pe", "act", "dve", "pool", "sp")

        def tiny(eng, col):
            if eng == "pe":
                return lambda: nc.tensor.matmul(self.ps[7][0:1, 0:1], lhsT=bar[0:1, 6:7], rhs=bar[0:1, 7:8], start=True, stop=True)
            if eng == "act":
                return lambda: nc.scalar.copy(out=bar[0:1, col:col + 1], in_=bar[0:1, 7:8])
            if eng == "dve":
                return lambda: nc.vector.tensor_copy(out=bar[0:1, col:col + 1], in_=bar[0:1, 7:8])
            if eng == "pool":
                return lambda: nc.gpsimd.tensor_copy(out=bar[0:1, col:col + 1], in_=bar[0:1, 7:8])
            return lambda: nc.sync.nop()

        for ci, e in enumerate(engs):
            w = [("barA", k, e)] + ([("ps", 7)] if e == "pe" else [])
            S.op(e, tiny(e, ci), w=w, alldma=(e == "sp"))
        for ci, e in enumerate(engs):
            w = [("barB", k, e)] + ([("ps", 7)] if e == "pe" else [])
            S.op(e, tiny(e, ci), r=[("barA", k, e2) for e2 in engs if e2 != e], w=w)

    def end_phase(self):
        self.barrier()
        self.S.flush()
        self.pes.close()
        self.pes = ExitStack()
        self.wb = []
        self.nphase += 1

    def finish(self):
        self.end_phase()
        st = self.S.stats()
        self.es.close()
        return st


class RowSplit:
    def __init__(self, nc, name, bounds, ncols, dt, kind="Internal"):
        self.bounds = list(bounds)
        self.parts = [nc.dram_tensor(f"{name}_{i}", [bounds[i + 1] - bounds[i], ncols], dt, kind=kind).ap() for i in range(len(bounds) - 1)]

    def rows(self, r0, r1):
        for i in range(len(self.parts)):
            if self.bounds[i] <= r0 and r1 <= self.bounds[i + 1]:
                return self.parts[i][r0 - self.bounds[i]:r1 - self.bounds[i], :]
        raise AssertionError((r0, r1, self.bounds))


def gemm(cx, jobs_blocks, xsrc, T, epilogue):
    nc, S = cx.nc, cx.S
    for bi, blk in enumerate(jobs_blocks):
        lbufs = []
        for (wap, K) in blk["loads"]:
            wi = cx.next_wb()
            KC = K // 128
            ncols = wap.shape[1]
            dst = cx.wb[wi][:, 0:KC, 0:ncols]
            src = wap.rearrange("(c p) n -> p c n", p=128)
            S.op("pool", (lambda dst=dst, src=src: nc.gpsimd.dma_start(out=dst, in_=src)), w=[("wb", wi)], dma=True)
            lbufs.append((wi, KC))
        for ji, job in enumerate(blk["jobs"]):
            for t in range(T // NT5):
                accs = []
                for (li, c0, width, xkey) in job:
                    wi, KC = lbufs[li]
                    xap, xres, xKC = xsrc[xkey]
                    assert xKC == KC
                    pi = cx.next_ps()
                    pst = cx.ps[pi][0:width, :]
                    for k in range(KC):
                        S.op("pe", (lambda pst=pst, wi=wi, k=k, c0=c0, width=width, xap=xap, t=t, KC=KC:
                                    nc.tensor.matmul(pst, lhsT=cx.wb[wi][:, k, c0:c0 + width],
                                                     rhs=xap[:, k, t * NT5:(t + 1) * NT5],
                                                     start=(k == 0), stop=(k == KC - 1))),
                             r=[("wb", wi)] + list(xres), w=[("ps", pi)])
                    accs.append((pst, ("ps", pi)))
                epilogue(bi, ji, t, accs)


def col_blocks(cx, w_ap_list, ncols, K, xkeys):
    jb = []
    c0 = 0
    while c0 < ncols:
        c1 = min(ncols, c0 + cx.wcols)
        jobs, meta = [], []
        o = 0
        while o < c1 - c0:
            wd = min(128, c1 - c0 - o)
            jobs.append([(li, o, wd, xkeys[li]) for li in range(len(w_ap_list))])
            meta.append((c0 + o, wd))
            o += wd
        jb.append(dict(loads=[(wap[:, c0:c1], K) for wap in w_ap_list], jobs=jobs, meta=meta))
        c0 = c1
    return jb


def load_x_norm(cx, xT_ap, tok0, T, gtile, xg, xg_res, xst, xsq, ones_f, rstd, src_res=(), router=None, tag="x", want_xg=True):
    nc, S = cx.nc, cx.S
    KC = D // 128
    PC = 2
    src = xT_ap.rearrange("(c p) t -> p c t", p=128)
    nt = T // NT5
    pss = [cx.next_ps() for _ in range(nt)]
    rps = [cx.next_ps() for _ in range(nt)] if router is not None else None
    npieces = KC // PC
    for pc in range(npieces):
        si = pc % len(xst)
        st = xst[si]
        S.op("sp", (lambda st=st, pc=pc: nc.sync.dma_start(out=st[:, :, 0:T], in_=src[:, pc * PC:(pc + 1) * PC, tok0:tok0 + T])),
             r=list(src_res), w=[(tag + "st", si)], dma=True)
        if router is not None:
            xgf, wr_sb = router["xgf"], router["wr"]
            for c in range(PC):
                ch = pc * PC + c
                S.op("pool", (lambda st=st, c=c, ch=ch: nc.gpsimd.tensor_scalar(out=xgf[:, c, 0:T], in0=st[:, c, 0:T],
                                                                               scalar1=gtile[:, ch:ch + 1], scalar2=None, op0=ALU.mult)),
                     r=[(tag + "st", si), ("gtile",)], w=[("xgf",)])
            for c in range(PC):
                ch = pc * PC + c
                for t in range(nt):
                    first = (pc == 0 and c == 0)
                    last = (pc == npieces - 1 and c == PC - 1)
                    S.op("pe", (lambda t=t, c=c, ch=ch, first=first, last=last:
                                nc.tensor.matmul(cx.ps[rps[t]][0:NEXP, :], lhsT=wr_sb[:, ch, :], rhs=xgf[:, c, t * NT5:(t + 1) * NT5],
                                                 start=first, stop=last)),
                         r=[("xgf",), ("wr",)], w=[("ps", rps[t])])
        S.op("act", (lambda st=st: nc.scalar.activation(out=xsq[:, :, 0:T], in_=st[:, :, 0:T], func=AF.Square)),
             r=[(tag + "st", si)], w=[(tag + "sq",)])
        for c in range(PC):
            for t in range(nt):
                first = (pc == 0 and c == 0)
                last = (pc == npieces - 1 and c == PC - 1)
                S.op("pe", (lambda t=t, c=c, first=first, last=last:
                            nc.tensor.matmul(cx.ps[pss[t]][:, :], lhsT=ones_f[:, :], rhs=xsq[:, c, t * NT5:(t + 1) * NT5],
                                             start=first, stop=last)),
                     r=[(tag + "sq",), ("ones",)], w=[("ps", pss[t])])
        for c in (range(PC) if want_xg else ()):
            ch = pc * PC + c
            S.op("dve", (lambda st=st, c=c, ch=ch: nc.vector.tensor_scalar(out=xg[:, ch, 0:T], in0=st[:, c, 0:T],
                                                                        scalar1=gtile[:, ch:ch + 1], scalar2=None, op0=ALU.mult)),
                 r=[(tag + "st", si), ("gtile",)], w=list(xg_res))
    for t in range(nt):
        sl = rstd[:, t * NT5:(t + 1) * NT5]
        S.op("dve", (lambda sl=sl, t=t: nc.vector.tensor_scalar(out=sl, in0=cx.ps[pss[t]][:, :], scalar1=1.0 / D, scalar2=EPS,
                                                             op0=ALU.mult, op1=ALU.add)),
             r=[("ps", pss[t])], w=[("rstd",)])
        S.op("act", (lambda sl=sl: nc.scalar.activation(out=sl, in_=sl, func=AF.Sqrt)), r=[("rstd",)], w=[("rstd",)])
        S.op("dve", (lambda sl=sl: nc.vector.reciprocal(out=sl, in_=sl)), r=[("rstd",)], w=[("rstd",)])
    if router is not None:
        lgT = router["lgT"]
        for t in range(nt):
            S.op("dve", (lambda t=t: nc.vector.tensor_tensor(out=lgT[0:NEXP, t * NT5:(t + 1) * NT5], in0=cx.ps[rps[t]][0:NEXP, :],
                                                             in1=rstd[0:NEXP, t * NT5:(t + 1) * NT5], op=ALU.mult)),
                 r=[("ps", rps[t]), ("rstd",)], w=[("lgT",)])


def a_subblocks():
    subs = []

    def rng(r0, r1, kind):
        r = r0
        while r < r1:
            wd = min(128, r1 - r)
            subs.append((r, wd, kind))
            r += wd
    rng(R_HQ, R_HF, "silu")
    rng(R_HF, R_HI, "hf")
    rng(R_HI, R_HOG, "id")
    rng(R_HOG, R_CQ, "silu")
    rng(R_CQ, R_GA, "id")
    rng(R_GA, IN_COLS, "sig")
    return subs


def group_blocks(subs, wcols):
    blocks, cur = [], []
    for s in subs:
        if cur and (s[0] + s[1] - cur[0][0] > wcols or s[0] != cur[-1][0] + cur[-1][1]):
            blocks.append(cur)
            cur = []
        cur.append(s)
    if cur:
        blocks.append(cur)
    return blocks


def phase_A(cx, layer, xT, w_in, g_in, lbl, projT):
    nc, S = cx.nc, cx.S
    cx.alloc_wb(3, 512)
    xg = cx.sb("xg", [128, 32, TT], BF16)
    xst = [cx.sb(f"xst{i}", [128, 2, TT], F32) for i in range(2)]
    xsq = cx.sb("xsq", [128, 2, TT], F32)
    ones_f = cx.sb("ones_f", [128, 128], F32)
    gtile = cx.sb("gtile", [128, 32], F32)
    lb = cx.sb("lb", [128, 16], F32)
    oml = cx.sb("oml", [128, 16], F32)
    lbt = cx.sb("lbt", [128, 2, 16], F32)
    rstd = cx.sb("rstd", [128, TT], F32)
    NEZ, NEO = 2, 3
    ez = [cx.sb(f"ez{i}", [128, NT5], F32) for i in range(NEZ)]
    eo = [cx.sb(f"eo{i}", [128, NT5], F32) for i in range(NEO)]
    cnt = {"ez": 0, "eo": 0}

    S.op("pool", lambda: nc.gpsimd.memset(ones_f[:, :], 1.0), w=[("ones",)])
    S.op("sp", lambda: nc.sync.dma_start(out=gtile[:, :], in_=g_in), w=[("gtile",)], dma=True)
    if layer == 0:
        S.op("pool", lambda: nc.gpsimd.memset(lb[:, :], 0.0), w=[("lb",)])
        S.op("pool", lambda: nc.gpsimd.memset(oml[:, :], 1.0), w=[("oml",)])
    else:
        assert layer == 1
        S.op("sp", lambda: nc.sync.dma_start(out=lbt[:, :, :], in_=lbl.rearrange("d p c -> p d c")), w=[("lbt",)], dma=True)
        S.op("dve", lambda: nc.vector.tensor_sub(out=oml[:, :], in0=lbt[:, 1, :], in1=lbt[:, 0, :]), r=[("lbt",)], w=[("oml",)])
        S.op("act", lambda: nc.scalar.activation(out=lb[:, :], in_=oml[:, :], func=AF.Sigmoid), r=[("oml",)], w=[("lb",)])
        S.op("dve", lambda: nc.vector.tensor_scalar(out=oml[:, :], in0=lb[:, :], scalar1=-1.0, scalar2=1.0, op0=ALU.mult, op1=ALU.add),
             r=[("lb",)], w=[("oml",)])

    subs = a_subblocks()
    blocks = group_blocks(subs, cx.wcols)
    jb = []
    for blk in blocks:
        c0 = blk[0][0]
        c1 = blk[-1][0] + blk[-1][1]
        jb.append(dict(loads=[(w_in[:, c0:c1], D)], jobs=[[(0, s[0] - c0, s[1], "x")] for s in blk], meta=blk))

    for tt in range(SB // TT):
        tok0 = tt * TT
        load_x_norm(cx, xT, tok0, TT, gtile, xg, [("xg",)], xst, xsq, ones_f, rstd)

        def epi(bi, ji, t, accs, tok0=tok0):
            row0, width, kind = jb[bi]["meta"][ji]
            pst, pres = accs[0]
            zi = cnt["ez"] % NEZ
            cnt["ez"] += 1
            z = ez[zi][0:width, :]
            rs = rstd[0:width, t * NT5:(t + 1) * NT5]
            S.op("dve", lambda: nc.vector.tensor_tensor(out=z, in0=pst, in1=rs, op=ALU.mult), r=[pres, ("rstd",)], w=[("ez", zi)])
            cols = slice(tok0 + t * NT5, tok0 + (t + 1) * NT5)

            def store(tile_ap, res, r0):
                S.op("sp", lambda: nc.sync.dma_start(out=projT.rows(r0, r0 + width)[:, cols], in_=tile_ap), r=[res], w=[("projT", r0, cols.start)], dma=True)

            def new_eo():
                oi = cnt["eo"] % NEO
                cnt["eo"] += 1
                return oi, eo[oi][0:width, :]

            if kind == "id":
                store(z, ("ez", zi), row0)
            elif kind in ("silu", "sig"):
                oi, o = new_eo()
                fn = AF.Silu if kind == "silu" else AF.Sigmoid
                S.op("act", lambda: nc.scalar.activation(out=o, in_=z, func=fn), r=[("ez", zi)], w=[("eo", oi)])
                store(o, ("eo", oi), row0)
            else:
                ch = (row0 - R_HF) // 128
                oi, sg = new_eo()
                S.op("act", lambda: nc.scalar.activation(out=sg, in_=z, func=AF.Sigmoid), r=[("ez", zi)], w=[("eo", oi)])
                S.op("dve", lambda: nc.vector.tensor_scalar(out=z, in0=sg, scalar1=oml[0:width, ch:ch + 1], scalar2=lb[0:width, ch:ch + 1],
                                                             op0=ALU.mult, op1=ALU.add),
                     r=[("eo", oi), ("oml",), ("lb",)], w=[("ez", zi)])
                oi2, gl = new_eo()
                S.op("act", lambda: nc.scalar.activation(out=gl, in_=z, func=AF.Ln), r=[("ez", zi)], w=[("eo", oi2)])
                store(gl, ("eo", oi2), row0)
                oi3, kk = new_eo()
                S.op("dve", lambda: nc.vector.tensor_scalar(out=kk, in0=z, scalar1=-1.0, scalar2=1.0, op0=ALU.mult, op1=ALU.add),
                     r=[("ez", zi)], w=[("eo", oi3)])
                store(kk, ("eo", oi3), R_K + (row0 - R_HF))

        gemm(cx, jb, {"x": (xg, [("xg",)], 32)}, TT, epi)
    cx.end_phase()


def phase_T(cx, pairs):
    nc, S = cx.nc, cx.S
    ident = cx.sb("ident", [128, 128], F32)
    tin = [cx.sb(f"tin{i}", [128, 4, NT5], F32) for i in range(2)]
    tout = [cx.sb(f"tout{i}", [128, NT5], F32) for i in range(4)]
    S.op("pool", lambda: nc.gpsimd.iota(ident[:, :], pattern=[[1, 128]], base=0, channel_multiplier=-1, allow_small_or_imprecise_dtypes=True),
         w=[("ident",)])
    S.op("dve", lambda: nc.vector.tensor_scalar(out=ident[:, :], in0=ident[:, :], scalar1=0.0, scalar2=None, op0=ALU.is_equal),
         r=[("ident",)], w=[("ident",)])
    it = 0
    no = 0
    for (src, dst) in pairs:
        R, C = src.shape
        for r0 in range(0, R, 512):
            for c0 in range(0, C, 512):
                ti = it % 2
                it += 1
                tl = tin[ti]
                S.op("sp", (lambda tl=tl, src=src, r0=r0, c0=c0: nc.sync.dma_start(
                    out=tl[:, :, :], in_=src[r0:r0 + 512, c0:c0 + 512].rearrange("(j p) c -> p j c", p=128))), w=[("tin", ti)], dma=True)
                for cb in range(4):
                    pi = cx.next_ps()
                    for j in range(4):
                        S.op("pe", (lambda tl=tl, cb=cb, j=j, pi=pi: nc.tensor.transpose(out=cx.ps[pi][:, j * 128:(j + 1) * 128],
                                                                                   in_=tl[:, j, cb * 128:(cb + 1) * 128], identity=ident[:, :])),
                             r=[("tin", ti), ("ident",)], w=[("ps", pi)])
                    oi = no % 4
                    no += 1
                    to = tout[oi]
                    if no % 2 == 0:
                        S.op("dve", (lambda to=to, pi=pi: nc.vector.tensor_copy(out=to[:, :], in_=cx.ps[pi][:, :])), r=[("ps", pi)], w=[("tout", oi)])
                    else:
                        S.op("act", (lambda to=to, pi=pi: nc.scalar.copy(out=to[:, :], in_=cx.ps[pi][:, :])), r=[("ps", pi)], w=[("tout", oi)])
                    S.op("sp", (lambda to=to, dst=dst, cb=cb, r0=r0, c0=c0: nc.sync.dma_start(out=dst[c0 + cb * 128:c0 + (cb + 1) * 128, r0:r0 + 512], in_=to[:, :])),
                         r=[("tout", oi)], w=[("tdst",)], dma=True)
    cx.end_phase()


def phase_Bh(cx, projT, tokG, tokK, tokV, tokOG, gain_all, oa_all):
    nc, S = cx.nc, cx.S
    HW_ = HPC * 128
    SS = 256
    NJ = SS // CHUNK
    NSS = SB // SS
    FW = HPC * SS
    fm = {n: [cx.sb(f"{n}{i}", [128, FW], F32) for i in range(2)] for n in ("q", "g", "k")}
    tm = {n: [cx.sb(f"{n}t{i}", [64, NJ, HW_], F32) for i in range(2)] for n in ("g", "k", "v", "og")}
    bT = cx.sb("bT", [128, FW], F32)
    dd = cx.sb("dd", [128, FW], F32)
    e1 = cx.sb("e1", [128, FW], F32)
    qe = cx.sb("qe", [128, FW], BF16)
    ke = cx.sb("ke", [128, FW], BF16)
    ebm = cx.sb("ebm", [128, HPC * NJ], F32)
    ebl = cx.sb("ebl", [128, HPC * NJ], F32)
    ex = [cx.sb(f"ex{i}", [64, HW_], F32) for i in range(2)]
    kd = cx.sb("kd", [64, NJ, HW_], BF16)
    v_bf = cx.sb("v_bf", [64, NJ, HW_], BF16)
    gg = cx.sb("gg", [64, NJ, HW_], F32)
    o_all = cx.sb("o_all", [64, NJ, HW_], F32)
    sq = cx.sb("sq", [64, NJ, HW_], F32)
    ssum = cx.sb("ssum", [64, NJ * HPC], F32)
    P = cx.sb("P", [64, HPC, CHUNK], BF16)
    St = cx.sb("St", [128, HPC, 128], F32)
    S_bf = cx.sb("S_bf", [128, HPC, 128], BF16)
    segmask = cx.sb("segmask", [128, FW], F32)
    tri = cx.sb("tri", [64, CHUNK], F32)
    U = cx.sb("U", [64, CHUNK], F32)
    gain_bc = cx.sb("gain_bc", [64, HW_], F32)

    S.op("pool", lambda: nc.gpsimd.iota(tri[:, :], pattern=[[1, CHUNK]], base=0, channel_multiplier=-1, allow_small_or_imprecise_dtypes=True), w=[("tri",)])
    S.op("dve", lambda: nc.vector.tensor_scalar(out=U[:, :], in0=tri[:, :], scalar1=0.0, scalar2=None, op0=ALU.is_lt), r=[("tri",)], w=[("U",)])
    S.op("dve", lambda: nc.vector.tensor_scalar(out=tri[:, :], in0=tri[:, :], scalar1=0.0, scalar2=None, op0=ALU.is_ge), r=[("tri",), ("U",)], w=[("tri",)])
    S.op("pool", lambda: nc.gpsimd.iota(segmask[:, :], pattern=[[0, FW // CHUNK], [1, CHUNK]], base=0, channel_multiplier=0, allow_small_or_imprecise_dtypes=True), w=[("seg",)])
    S.op("dve", lambda: nc.vector.tensor_scalar(out=segmask[:, :], in0=segmask[:, :], scalar1=0.0, scalar2=None, op0=ALU.is_gt),
         r=[("seg",)], w=[("seg",)])
    for hg in range(NHG):
        fsl = slice(hg * HW_, (hg + 1) * HW_)
        qT = projT.rows(R_HQ + hg * HW_, R_HQ + (hg + 1) * HW_)
        gT = projT.rows(R_HF + hg * HW_, R_HF + (hg + 1) * HW_)
        kT = projT.rows(R_K + hg * HW_, R_K + (hg + 1) * HW_)
        g_tok, k_tok, v_tok, og_tok = tokG[:, fsl], tokK[:, fsl], tokV[:, fsl], tokOG[:, fsl]
        gain = gain_all[0:1, fsl]
        oa = oa_all[:, fsl]
        S.op("sp", (lambda gain=gain: nc.sync.dma_start(out=gain_bc[:, :], in_=gain.to_broadcast([64, HW_]))), w=[("gain",)], dma=True)
        S.op("pool", lambda: nc.gpsimd.memset(St[:, :, :], 0.0), w=[("S",)])

        PX, PSC, PO, PDS = (0, 1), (2, 3), (4, 5), (6, 7)
        nchunk = 0
        for ss in range(NSS):
            t0 = ss * SS
            bi = ss % 2
            for n, srcT in (("q", qT), ("g", gT), ("k", kT)):
                dst = fm[n][bi]
                S.op("sp", (lambda dst=dst, srcT=srcT, t0=t0: nc.sync.dma_start(out=dst[:, :].rearrange("p (h t) -> p h t", h=HPC),
                                                                                in_=srcT.rearrange("(h c) t -> c h t", c=128)[:, :, t0:t0 + SS])),
                     w=[("fm", n, bi)], dma=True)
            for n, srcT in (("g", g_tok), ("k", k_tok), ("v", v_tok), ("og", og_tok)):
                dst = tm[n][bi]
                S.op("sp", (lambda dst=dst, srcT=srcT, t0=t0: nc.sync.dma_start(out=dst[:, :, :], in_=srcT[t0:t0 + SS, :].rearrange("(j p) f -> p j f", p=64))),
                     w=[("tm", n, bi)], dma=True)
            qs, gs, ks = fm["q"][bi], fm["g"][bi], fm["k"][bi]
            gt, kt, vt, ogt = tm["g"][bi], tm["k"][bi], tm["v"][bi], tm["og"][bi]
            S.op("dve", (lambda gs=gs: nc.vector.tensor_tensor_scan(out=bT[:, :], data0=segmask[:, :], data1=gs[:, :], initial=0.0, op0=ALU.mult, op1=ALU.add)),
                 r=[("fm", "g", bi), ("seg",)], w=[("bT",)])
            bT3 = bT[:, :].rearrange("p (n l) -> p n l", l=CHUNK)
            S.op("dve", lambda: nc.vector.tensor_tensor(out=dd[:, :].rearrange("p (n l) -> p n l", l=CHUNK), in0=bT3,
                                                        in1=bT3[:, :, 31:32].to_broadcast([128, HPC * NJ, CHUNK]), op=ALU.subtract),
                 r=[("bT",)], w=[("dd",)])
            S.op("act", lambda: nc.scalar.activation(out=ebm[:, :], in_=bT3[:, :, 31], func=AF.Exp), r=[("bT",)], w=[("ebm",)])
            S.op("act", lambda: nc.scalar.activation(out=ebl[:, :], in_=bT3[:, :, CHUNK - 1], func=AF.Exp), r=[("bT",)], w=[("ebl",)])
            S.op("act", lambda: nc.scalar.activation(out=e1[:, :], in_=dd[:, :], func=AF.Exp), r=[("dd",)], w=[("e1",)])
            S.op("dve", (lambda qs=qs: nc.vector.tensor_tensor(out=qe[:, :], in0=qs[:, :], in1=e1[:, :], op=ALU.mult)), r=[("fm", "q", bi), ("e1",)], w=[("qe",)])
            S.op("act", lambda: nc.scalar.activation(out=e1[:, :], in_=dd[:, :], func=AF.Exp, scale=-1.0), r=[("dd",), ("qe",)], w=[("e1",)])
            S.op("pool", (lambda ks=ks: nc.gpsimd.tensor_tensor(out=ke[:, :], in0=ks[:, :], in1=e1[:, :], op=ALU.mult)), r=[("fm", "k", bi), ("e1",)], w=[("ke",)])
            S.op("act", (lambda vt=vt: nc.scalar.copy(out=v_bf[:, :, :], in_=vt[:, :, :])), r=[("tm", "v", bi)], w=[("v_bf",)])
            S.op("pool", (lambda ogt=ogt: nc.gpsimd.tensor_tensor(out=gg[:, :, :], in0=ogt[:, :, :], in1=gain_bc[:, :].unsqueeze(1).to_broadcast([64, NJ, HW_]), op=ALU.mult)),
                 r=[("tm", "og", bi), ("gain",)], w=[("gg",)])
            for j in range(NJ):
                px = PX[j % 2]
                S.op("pe", (lambda j=j, px=px, gt=gt: nc.tensor.matmul(cx.ps[px][0:64, :], lhsT=U[:, :], rhs=gt[:, j, :], start=True, stop=True)),
                     r=[("tm", "g", bi), ("U",)], w=[("ps", px)])
                exj = ex[j % 2]
                S.op("act", (lambda px=px, exj=exj: nc.scalar.activation(out=exj[:, :], in_=cx.ps[px][0:64, :], func=AF.Exp)), r=[("ps", px)], w=[("ex", j % 2)])
                S.op("pool", (lambda j=j, exj=exj, kt=kt: nc.gpsimd.tensor_tensor(out=kd[:, j, :], in0=kt[:, j, :], in1=exj[:, :], op=ALU.mult)),
                     r=[("tm", "k", bi), ("ex", j % 2)], w=[("kd", j)])
            for j in range(NJ):
                cs = slice(j * CHUNK, (j + 1) * CHUNK)
                psc, po, pds = PSC[nchunk % 2], PO[nchunk % 2], PDS[nchunk % 2]
                nchunk += 1
                for h in range(HPC):
                    S.op("act", (lambda h=h, j=j: nc.scalar.activation(out=S_bf[:, h, :], in_=St[:, h, :], func=AF.Copy, scale=ebm[:, h * NJ + j:h * NJ + j + 1])),
                         r=[("S",), ("ebm",)], w=[("S_bf",)])
                for h in range(HPC):
                    fs = slice(h * SS + j * CHUNK, h * SS + (j + 1) * CHUNK)
                    S.op("pe", (lambda h=h, fs=fs, psc=psc: nc.tensor.matmul(cx.ps[psc][0:64, h * CHUNK:(h + 1) * CHUNK], lhsT=ke[:, fs], rhs=qe[:, fs], start=True, stop=True)),
                         r=[("ke",), ("qe",)], w=[("ps", psc)])
                S.op("dve", (lambda psc=psc: nc.vector.tensor_tensor(out=P[:, :, :], in0=cx.ps[psc][0:64, 0:HPC * CHUNK].rearrange("p (h t) -> p h t", h=HPC),
                                                                     in1=tri[:, :].unsqueeze(1).to_broadcast([64, HPC, CHUNK]), op=ALU.mult)),
                     r=[("ps", psc), ("tri",)], w=[("P",)])
                for h in range(HPC):
                    fs = slice(h * SS + j * CHUNK, h * SS + (j + 1) * CHUNK)
                    hs = slice(h * 128, (h + 1) * 128)
                    S.op("pe", (lambda h=h, fs=fs, hs=hs, po=po: nc.tensor.matmul(cx.ps[po][0:64, hs], lhsT=qe[:, fs], rhs=S_bf[:, h, :], start=True, stop=False)),
                         r=[("qe",), ("S_bf",)], w=[("ps", po)])
                    S.op("pe", (lambda h=h, j=j, hs=hs, po=po: nc.tensor.matmul(cx.ps[po][0:64, hs], lhsT=P[:, h, :], rhs=v_bf[:, j, hs], start=False, stop=True)),
                         r=[("P",), ("v_bf",)], w=[("ps", po)])
                for h in range(HPC):
                    hs = slice(h * 128, (h + 1) * 128)
                    S.op("pe", (lambda j=j, hs=hs, pds=pds: nc.tensor.matmul(cx.ps[pds][:, hs], lhsT=kd[:, j, hs], rhs=v_bf[:, j, hs], start=True, stop=True)),
                         r=[("kd", j), ("v_bf",)], w=[("ps", pds)])
                for h in range(HPC):
                    hs = slice(h * 128, (h + 1) * 128)
                    S.op("dve", (lambda h=h, j=j, hs=hs, pds=pds: nc.vector.scalar_tensor_tensor(out=St[:, h, :], in0=St[:, h, :], scalar=ebl[:, h * NJ + j:h * NJ + j + 1],
                                                                                              in1=cx.ps[pds][:, hs], op0=ALU.mult, op1=ALU.add)),
                         r=[("S",), ("ebl",), ("ps", pds), ("S_bf",)], w=[("S",)])
                S.op("act", (lambda j=j, po=po: nc.scalar.copy(out=o_all[:, j, :], in_=cx.ps[po][0:64, :])), r=[("ps", po)], w=[("o_all", j)])
            oall_res = [("o_all", j) for j in range(NJ)]
            S.op("pool", lambda: nc.gpsimd.tensor_tensor(out=sq[:, :, :], in0=o_all[:, :, :], in1=o_all[:, :, :], op=ALU.mult), r=oall_res, w=[("sq",)])
            S.op("dve", lambda: nc.vector.reduce_sum(out=ssum[:, :], in_=sq[:, :, :].rearrange("p j (h v) -> p (j h) v", v=128), axis=AX.X), r=[("sq",)], w=[("ssum",)])
            S.op("dve", lambda: nc.vector.tensor_scalar(out=ssum[:, :], in0=ssum[:, :], scalar1=1.0 / 128, scalar2=EPS, op0=ALU.mult, op1=ALU.add), r=[("ssum",)], w=[("ssum",)])
            S.op("act", lambda: nc.scalar.activation(out=ssum[:, :], in_=ssum[:, :], func=AF.Sqrt), r=[("ssum",)], w=[("ssum",)])
            S.op("dve", lambda: nc.vector.reciprocal(out=ssum[:, :], in_=ssum[:, :]), r=[("ssum",)], w=[("ssum",)])
            S.op("dve", lambda: nc.vector.tensor_tensor(out=sq[:, :, :].rearrange("p j (h v) -> p (j h) v", v=128),
                                                        in0=o_all[:, :, :].rearrange("p j (h v) -> p (j h) v", v=128),
                                                        in1=ssum[:, :].unsqueeze(2).to_broadcast([64, NJ * HPC, 128]), op=ALU.mult),
                 r=oall_res + [("ssum",), ("sq",)], w=[("sq",)])
            S.op("pool", lambda: nc.gpsimd.tensor_tensor(out=sq[:, :, :], in0=sq[:, :, :], in1=gg[:, :, :], op=ALU.mult), r=[("sq",), ("gg",)], w=[("sq",)])
            S.op("sp", (lambda t0=t0, oa=oa: nc.sync.dma_start(out=oa[t0:t0 + SS, :].rearrange("(j p) f -> p j f", p=64), in_=sq[:, :, :])),
                 r=[("sq",)], w=[("out", "oa_tok")], dma=True)

    cx.end_phase()


def phase_Bm(cx, cqT, ckvT, kpeT, pos, qg_in, kvg_in, wuq_l, wukv_l, obT_all, QN, QP, KN, VV):
    nc, S = cx.nc, cx.S
    QC, KC = QR // 128, KVR // 128
    wuq_sb = cx.sb("wuq_sb", [128, QC, HPC * 192], BF16)
    wukv_sb = cx.sb("wukv_sb", [128, KC, HPC * 256], BF16)
    qg = cx.sb("qg", [128, QC], F32)
    kvg = cx.sb("kvg", [128, KC], F32)
    ones_f = cx.sb("ones_f", [128, 128], F32)
    ones_b = cx.sb("ones_b", [128, 128], BF16)
    permT = cx.sb("permT", [64, 64], F32)
    invf = cx.sb("invf", [64, 1], F32)
    kp_sb = cx.sb("kp_sb", [64, SB], BF16)
    masks = cx.sb("masks", [128, 4, NT5], BF16)
    cq = cx.sb("cq", [128, QC, NT5], F32)
    cqs = cx.sb("cqs", [128, QC, NT5], F32)
    cqn = cx.sb("cqn", [128, QC, NT5], BF16)
    ckv = cx.sb("ckv", [128, KC, NT5], F32)
    ckvs = cx.sb("ckvs", [128, KC, NT5], F32)
    ckvn = cx.sb("ckvn", [128, KC, NT5], BF16)
    rq = cx.sb("rq", [128, NT5], F32)
    rkv = cx.sb("rkv", [128, NT5], F32)
    rkv_tok = cx.sb("rkv_tok", [128, 4], F32)
    posf = cx.sb("posf", [64, NT5], F32)
    ang = cx.sb("ang", [64, NT5], F32)
    cos2 = cx.sb("cos2", [64, NT5], F32)
    sin2 = cx.sb("sin2", [64, NT5], F32)
    pe_f = [cx.sb(f"pe_f{i}", [64, NT5], F32) for i in range(2)]
    pe_t = [cx.sb(f"pe_t{i}", [64, NT5], F32) for i in range(2)]
    ob16 = [cx.sb(f"ob16_{i}", [128, NT5], BF16) for i in range(3)]
    of32 = [cx.sb(f"of32_{i}", [128, NT5], F32) for i in range(2)]
    cnt = {"pef": 0, "pet": 0, "ob16": 0, "of32": 0}

    def rot(name, pool):
        i = cnt[name] % len(pool)
        cnt[name] += 1
        return pool[i], (name, i)

    S.op("pool", lambda: nc.gpsimd.memset(ones_f[:, :], 1.0), w=[("ones_f",)])
    S.op("pool", lambda: nc.gpsimd.memset(ones_b[:, :], 1.0), w=[("ones_b",)])
    S.op("sp", lambda: nc.sync.dma_start(out=qg[:, :], in_=qg_in), w=[("qg",)], dma=True)
    S.op("sp", lambda: nc.sync.dma_start(out=kvg[:, :], in_=kvg_in), w=[("kvg",)], dma=True)
    S.op("pool", lambda: nc.gpsimd.iota(permT[:, :], pattern=[[1, 64]], base=0, channel_multiplier=-1, allow_small_or_imprecise_dtypes=True),
         w=[("perm",)])
    S.op("dve", lambda: nc.vector.tensor_scalar(out=pe_t[0][:, 0:64], in0=permT[:, :], scalar1=32.0, scalar2=None, op0=ALU.is_equal),
         r=[("perm",)], w=[("perm1",)])
    S.op("dve", lambda: nc.vector.tensor_scalar(out=pe_t[1][:, 0:64], in0=permT[:, :], scalar1=-32.0, scalar2=None, op0=ALU.is_equal),
         r=[("perm",)], w=[("perm2",)])
    S.op("dve", lambda: nc.vector.tensor_sub(out=permT[:, :], in0=pe_t[0][:, 0:64], in1=pe_t[1][:, 0:64]), r=[("perm1",), ("perm2",)], w=[("perm",)])
    S.op("pool", lambda: nc.gpsimd.iota(invf[0:32, :], pattern=[[0, 1]], base=0, channel_multiplier=1, allow_small_or_imprecise_dtypes=True),
         w=[("invf",)])
    S.op("pool", lambda: nc.gpsimd.iota(invf[32:64, :], pattern=[[0, 1]], base=0, channel_multiplier=1, allow_small_or_imprecise_dtypes=True),
         w=[("invf",)])
    S.op("dve", lambda: nc.vector.tensor_scalar(out=invf[:, :], in0=invf[:, :], scalar1=-float(np.log(10000.0) / 32.0), scalar2=None,
                                                 op0=ALU.mult), r=[("invf",)], w=[("invf",)])
    S.op("act", lambda: nc.scalar.activation(out=invf[:, :], in_=invf[:, :], func=AF.Exp), r=[("invf",)], w=[("invf",)])
    S.op("pool", lambda: nc.gpsimd.memset(masks[:, :, :], 0.0), w=[("masks",)])
    for d in range(4):
        S.op("pool", (lambda d=d: nc.gpsimd.memset(masks[0:64, d, 128 * d:NT5], 1.0)), w=[("masks",)])
        if 128 * d + 64 < NT5:
            S.op("pool", (lambda d=d: nc.gpsimd.memset(masks[64:128, d, 128 * d + 64:NT5], 1.0)), w=[("masks",)])

    def ssq_rstd(src, sq, NCH, rt, tag, nfeat):
        S.op("act", lambda: nc.scalar.activation(out=sq[:, :, :], in_=src[:, :, :], func=AF.Square), r=[(tag,)], w=[(tag + "s",)])
        pi = cx.next_ps()
        for c in range(NCH):
            S.op("pe", (lambda c=c: nc.tensor.matmul(cx.ps[pi][:, :], lhsT=ones_f[:, :], rhs=sq[:, c, :], start=(c == 0), stop=(c == NCH - 1))),
                 r=[(tag + "s",), ("ones_f",)], w=[("ps", pi)])
        S.op("dve", lambda: nc.vector.tensor_scalar(out=rt[:, :], in0=cx.ps[pi][:, :], scalar1=1.0 / nfeat, scalar2=EPS, op0=ALU.mult, op1=ALU.add),
             r=[("ps", pi)], w=[(tag + "r",)])
        S.op("act", lambda: nc.scalar.activation(out=rt[:, :], in_=rt[:, :], func=AF.Sqrt), r=[(tag + "r",)], w=[(tag + "r",)])
        S.op("dve", lambda: nc.vector.reciprocal(out=rt[:, :], in_=rt[:, :]), r=[(tag + "r",)], w=[(tag + "r",)])

    def rope(src_f, src_res, dst_ap, dst_res):
        pi = cx.next_ps()
        S.op("pe", lambda: nc.tensor.matmul(cx.ps[pi][0:64, :], lhsT=permT[:, :], rhs=src_f[:, :], start=True, stop=True),
             r=[src_res, ("perm",)], w=[("ps", pi)])
        tmp, tres = rot("pet", pe_t)
        S.op("dve", lambda: nc.vector.tensor_tensor(out=tmp[:, :], in0=cx.ps[pi][0:64, :], in1=sin2[:, :], op=ALU.mult),
             r=[("ps", pi), ("sin2",)], w=[tres])
        S.op("pool", lambda: nc.gpsimd.tensor_tensor(out=src_f[:, :], in0=src_f[:, :], in1=cos2[:, :], op=ALU.mult),
             r=[src_res, ("cos2",)], w=[src_res])
        S.op("dve", lambda: nc.vector.tensor_tensor(out=dst_ap, in0=src_f[:, :], in1=tmp[:, :], op=ALU.add), r=[src_res, tres], w=[dst_res])

    PI = float(np.pi)
    MAGIC = 12582912.0
    C1 = 6.28125
    C2 = float(2.0 * np.pi - 6.28125)
    red = cx.sb("red", [64, NT5], F32)
    red2 = cx.sb("red2", [64, NT5], F32)

    def sincos(dst, dres, shift):
        S.op("dve", lambda: nc.vector.tensor_scalar(out=red2[:, :], in0=ang[:, :], scalar1=shift, scalar2=None, op0=ALU.add), r=[("ang",)], w=[("red2",)])
        S.op("dve", lambda: nc.vector.tensor_scalar(out=red[:, :], in0=red2[:, :], scalar1=1.0 / (2 * PI), scalar2=None, op0=ALU.mult), r=[("red2",)], w=[("red",)])
        S.op("dve", lambda: nc.vector.tensor_scalar(out=red[:, :], in0=red[:, :], scalar1=MAGIC, scalar2=None, op0=ALU.add), r=[("red",)], w=[("red",)])
        S.op("dve", lambda: nc.vector.tensor_scalar(out=red[:, :], in0=red[:, :], scalar1=MAGIC, scalar2=None, op0=ALU.subtract), r=[("red",)], w=[("red",)])
        S.op("dve", lambda: nc.vector.scalar_tensor_tensor(out=red2[:, :], in0=red[:, :], scalar=-C1, in1=red2[:, :], op0=ALU.mult, op1=ALU.add),
             r=[("red",), ("red2",)], w=[("red2",)])
        S.op("dve", lambda: nc.vector.scalar_tensor_tensor(out=red2[:, :], in0=red[:, :], scalar=-C2, in1=red2[:, :], op0=ALU.mult, op1=ALU.add),
             r=[("red",), ("red2",)], w=[("red2",)])
        S.op("dve", lambda: nc.vector.tensor_scalar(out=red2[:, :], in0=red2[:, :], scalar1=PI, scalar2=-PI, op0=ALU.min, op1=ALU.max), r=[("red2",)], w=[("red2",)])
        S.op("act", lambda: nc.scalar.activation(out=dst[:, :], in_=red2[:, :], func=AF.Sin), r=[("red2",)], w=[dres])

    NTL = SB // NT5
    kn_sb = [cx.sb(f"kn{i}", [128, SB], BF16) for i in range(2)]
    v_sb = [cx.sb(f"v{i}", [128, SB // 128, 128], BF16) for i in range(2)]
    qn_sb = [cx.sb(f"qn{i}", [128, NT5], BF16) for i in range(2)]
    qp_sb = [cx.sb(f"qp{i}", [64, NT5], BF16) for i in range(2)]
    pT = [cx.sb(f"pT{i}", [128, NT5], BF16) for i in range(3)]
    npt_box = [0]
    for hg in range(NHG):
        obT = obT_all[hg * HPC * MLA_DV:(hg + 1) * HPC * MLA_DV, :]
        wq_n = wuq_l[:, hg * HPC * 128:(hg + 1) * HPC * 128].rearrange("(c p) n -> p c n", p=128)
        wq_r = wuq_l[:, 16 * 128 + hg * HPC * 64:16 * 128 + (hg + 1) * HPC * 64].rearrange("(c p) n -> p c n", p=128)
        wk_n = wukv_l[:, hg * HPC * 128:(hg + 1) * HPC * 128].rearrange("(c p) n -> p c n", p=128)
        wk_v = wukv_l[:, 16 * 128 + hg * HPC * 128:16 * 128 + (hg + 1) * HPC * 128].rearrange("(c p) n -> p c n", p=128)
        S.op("pool", (lambda wq_n=wq_n: nc.gpsimd.dma_start(out=wuq_sb[:, :, 0:HPC * 128], in_=wq_n)), w=[("wuq",)], dma=True)
        S.op("pool", (lambda wq_r=wq_r: nc.gpsimd.dma_start(out=wuq_sb[:, :, HPC * 128:HPC * 192], in_=wq_r)), w=[("wuq",)], dma=True)
        S.op("pool", (lambda wk_n=wk_n: nc.gpsimd.dma_start(out=wukv_sb[:, :, 0:HPC * 128], in_=wk_n)), w=[("wukv",)], dma=True)
        S.op("pool", (lambda wk_v=wk_v: nc.gpsimd.dma_start(out=wukv_sb[:, :, HPC * 128:HPC * 256], in_=wk_v)), w=[("wukv",)], dma=True)
        for tl in range(SB // NT5):
            t0 = tl * NT5
            ts = slice(t0, t0 + NT5)
            S.op("pool", (lambda ts=ts: nc.gpsimd.dma_start(out=posf[:, :], in_=pos[0:1, ts].to_broadcast([64, NT5]))), w=[("posf",)], dma=True)
            S.op("dve", lambda: nc.vector.tensor_scalar(out=ang[:, :], in0=posf[:, :], scalar1=invf[:, 0:1], scalar2=None, op0=ALU.mult),
                 r=[("posf",), ("invf",)], w=[("ang",)])
            for (dst, dres, shift) in ((sin2, ("sin2",), 0.0), (cos2, ("cos2",), 0.5 * PI)):
                sincos(dst, dres, shift)
            S.op("sp", (lambda ts=ts: nc.sync.dma_start(out=cq[:, :, :], in_=cqT.rearrange("(c p) t -> p c t", p=128)[:, :, ts])), w=[("cq",)], dma=True)
            ssq_rstd(cq, cqs, QC, rq, "cq", QR)
            for c in range(QC):
                S.op("dve", (lambda c=c: nc.vector.tensor_scalar(out=cqn[:, c, :], in0=cq[:, c, :], scalar1=qg[:, c:c + 1], scalar2=None, op0=ALU.mult)),
                     r=[("cq",), ("qg",)], w=[("cqn",)])
            for h in range(HPC):
                pi = cx.next_ps()
                for c in range(QC):
                    S.op("pe", (lambda c=c, h=h, pi=pi: nc.tensor.matmul(cx.ps[pi][:, :], lhsT=wuq_sb[:, c, h * 128:(h + 1) * 128], rhs=cqn[:, c, :],
                                                                       start=(c == 0), stop=(c == QC - 1))),
                         r=[("cqn",), ("wuq",)], w=[("ps", pi)])
                ot, ores = rot("ob16", ob16)
                S.op("dve", (lambda pi=pi, ot=ot: nc.vector.tensor_tensor(out=ot[:, :], in0=cx.ps[pi][:, :], in1=rq[:, :], op=ALU.mult)),
                     r=[("ps", pi), ("cqr",)], w=[ores])
                S.op("sp", (lambda h=h, ot=ot, ts=ts: nc.sync.dma_start(out=QN[h, :, ts], in_=ot[:, :])), r=[ores], w=[("QN", h, tl)], dma=True)
                pj = cx.next_ps()
                for c in range(QC):
                    S.op("pe", (lambda c=c, h=h, pj=pj: nc.tensor.matmul(cx.ps[pj][0:64, :], lhsT=wuq_sb[:, c, HPC * 128 + h * 64:HPC * 128 + (h + 1) * 64],
                                                                       rhs=cqn[:, c, :], start=(c == 0), stop=(c == QC - 1))),
                         r=[("cqn",), ("wuq",)], w=[("ps", pj)])
                pf, pres = rot("pef", pe_f)
                S.op("dve", (lambda pj=pj, pf=pf: nc.vector.tensor_tensor(out=pf[:, :], in0=cx.ps[pj][0:64, :], in1=rq[0:64, :], op=ALU.mult)),
                     r=[("ps", pj), ("cqr",)], w=[pres])
                ot2, ores2 = rot("ob16", ob16)
                rope(pf, pres, ot2[0:64, :], ores2)
                S.op("sp", (lambda h=h, ot2=ot2, ts=ts: nc.sync.dma_start(out=QP[h, :, ts], in_=ot2[0:64, :])), r=[ores2], w=[("QP", h, tl)], dma=True)
            S.op("sp", (lambda ts=ts: nc.sync.dma_start(out=ckv[:, :, :], in_=ckvT.rearrange("(c p) t -> p c t", p=128)[:, :, ts])), w=[("ckv",)], dma=True)
            ssq_rstd(ckv, ckvs, KC, rkv, "ckv", KVR)
            for c in range(KC):
                S.op("dve", (lambda c=c: nc.vector.tensor_scalar(out=ckvn[:, c, :], in0=ckv[:, c, :], scalar1=kvg[:, c:c + 1], scalar2=None, op0=ALU.mult)),
                     r=[("ckv",), ("kvg",)], w=[("ckvn",)])
            for h in range(HPC):
                pi = cx.next_ps()
                for c in range(KC):
                    S.op("pe", (lambda c=c, h=h, pi=pi: nc.tensor.matmul(cx.ps[pi][:, :], lhsT=wukv_sb[:, c, h * 128:(h + 1) * 128], rhs=ckvn[:, c, :],
                                                                       start=(c == 0), stop=(c == KC - 1))),
                         r=[("ckvn",), ("wukv",)], w=[("ps", pi)])
                ot, ores = rot("ob16", ob16)
                S.op("dve", (lambda pi=pi, ot=ot: nc.vector.tensor_tensor(out=ot[:, :], in0=cx.ps[pi][:, :], in1=rkv[:, :], op=ALU.mult)),
                     r=[("ps", pi), ("ckvr",)], w=[ores])
                S.op("sp", (lambda h=h, ot=ot, ts=ts: nc.sync.dma_start(out=KN[h, :, ts], in_=ot[:, :])), r=[ores], w=[("KN", h, tl)], dma=True)
            pk = cx.next_ps()
            for tb in range(4):
                for c in range(KC):
                    S.op("pe", (lambda c=c, tb=tb: nc.tensor.matmul(cx.ps[pk][:, tb:tb + 1], lhsT=ckvs[:, c, tb * 128:(tb + 1) * 128], rhs=ones_f[:, 0:1],
                                                                   start=(c == 0), stop=(c == KC - 1))),
                         r=[("ckvs",), ("ones_f",)], w=[("ps", pk)])
            S.op("dve", lambda: nc.vector.tensor_scalar(out=rkv_tok[:, :], in0=cx.ps[pk][:, 0:4], scalar1=1.0 / KVR, scalar2=EPS, op0=ALU.mult, op1=ALU.add),
                 r=[("ps", pk)], w=[("rkt",)])
            S.op("act", lambda: nc.scalar.activation(out=rkv_tok[:, :], in_=rkv_tok[:, :], func=AF.Sqrt), r=[("rkt",)], w=[("rkt",)])
            S.op("dve", lambda: nc.vector.reciprocal(out=rkv_tok[:, :], in_=rkv_tok[:, :]), r=[("rkt",)], w=[("rkt",)])
            for tb in range(4):
                pi = cx.next_ps()
                for c in range(KC):
                    S.op("pe", (lambda c=c, tb=tb, pi=pi: nc.tensor.matmul(cx.ps[pi][:, :], lhsT=ckvn[:, c, tb * 128:(tb + 1) * 128],
                                                                         rhs=wukv_sb[:, c, HPC * 128:HPC * 256], start=(c == 0), stop=(c == KC - 1))),
                         r=[("ckvn",), ("wukv",)], w=[("ps", pi)])
                ot, ores = rot("ob16", ob16)
                S.op("act", (lambda pi=pi, ot=ot, tb=tb: nc.scalar.activation(out=ot[:, :], in_=cx.ps[pi][:, :], func=AF.Copy, scale=rkv_tok[:, tb:tb + 1])),
                     r=[("ps", pi), ("rkt",)], w=[ores])
                S.op("sp", (lambda ot=ot, tb=tb, t0=t0: nc.sync.dma_start(out=VV[t0 + tb * 128:t0 + (tb + 1) * 128, :], in_=ot[:, :])),
                     r=[ores], w=[("VV", tl)], dma=True)
            pf, pres = rot("pef", pe_f)
            S.op("sp", (lambda pf=pf, ts=ts: nc.sync.dma_start(out=pf[:, :], in_=kpeT[:, ts])), w=[pres], dma=True)
            rope(pf, pres, kp_sb[:, ts], ("kp", tl))
        npt = 0
        nq = 0
        for h in range(HPC):
            hb = h % 2
            S.op("sp", (lambda h=h, hb=hb: nc.sync.dma_start(out=kn_sb[hb][:, :], in_=KN[h, :, :])),
                 r=[("KN", h, tl) for tl in range(NTL)], w=[("kn", hb)], dma=True)
            for half in range(2):
                b0, b1 = half * 32, (half + 1) * 32
                S.op("sp", (lambda h=h, hb=hb, b0=b0, b1=b1: nc.sync.dma_start(
                    out=v_sb[hb][:, b0:b1, :], in_=VV[b0 * 128:b1 * 128, h * 128:(h + 1) * 128].rearrange("(b p) v -> p b v", p=128))),
                     r=[("VV", tl) for tl in range(NTL)], w=[("v", hb, half)], dma=True)
            for i in range(NTL):
                qb = nq % 2
                nq += 1
                ts = slice(i * NT5, (i + 1) * NT5)
                S.op("sp", (lambda h=h, qb=qb, ts=ts: nc.sync.dma_start(out=qn_sb[qb][:, :], in_=QN[h, :, ts])), r=[("QN", h, i)], w=[("qn", qb)], dma=True)
                S.op("sp", (lambda h=h, qb=qb, ts=ts: nc.sync.dma_start(out=qp_sb[qb][:, :], in_=QP[h, :, ts])), r=[("QP", h, i)], w=[("qp", qb)], dma=True)
                bo, bl = 2 + qb, 4 + qb
                nkb = 4 * (i + 1)

                def qk(kb, hb=hb, qb=qb):
                    bs = kb % 2
                    ks = slice(kb * 128, (kb + 1) * 128)
                    S.op("pe", lambda: nc.tensor.matmul(cx.ps[bs][:, :], lhsT=kn_sb[hb][:, ks], rhs=qn_sb[qb][:, :], start=True, stop=False),
                         r=[("kn", hb), ("qn", qb)], w=[("ps", bs)])
                    S.op("pe", lambda: nc.tensor.matmul(cx.ps[bs][:, :], lhsT=kp_sb[0:64, ks], rhs=qp_sb[qb][0:64, :], start=False, stop=True),
                         r=[("kp", kb // 4), ("qp", qb)], w=[("ps", bs)])

                qk(0)
                for kb in range(nkb):
                    if kb + 1 < nkb:
                        qk(kb + 1)
                    bs = kb % 2
                    pt = pT[npt % 3]
                    pres = ("pT", npt % 3)
                    npt += 1
                    S.op("act", (lambda bs=bs, pt=pt: nc.scalar.activation(out=pt[:, :], in_=cx.ps[bs][:, :], func=AF.Exp, scale=ATT_SCALE)),
                         r=[("ps", bs)], w=[pres])
                    if kb >= 4 * i:
                        d = kb - 4 * i
                        S.op("pool", (lambda pt=pt, d=d: nc.gpsimd.tensor_tensor(out=pt[:, :], in0=pt[:, :], in1=masks[:, d, :], op=ALU.mult)),
                             r=[pres, ("masks",)], w=[pres])
                    S.op("pe", (lambda kb=kb, pt=pt, hb=hb, bo=bo, nkb=nkb: nc.tensor.matmul(cx.ps[bo][:, :], lhsT=v_sb[hb][:, kb, :], rhs=pt[:, :],
                                                                                         start=(kb == 0), stop=(kb == nkb - 1))),
                         r=[("v", hb, kb // 32), pres], w=[("ps", bo)])
                    S.op("pe", (lambda kb=kb, pt=pt, bl=bl, nkb=nkb: nc.tensor.matmul(cx.ps[bl][:, :], lhsT=ones_b[:, :], rhs=pt[:, :],
                                                                                  start=(kb == 0), stop=(kb == nkb - 1))),
                         r=[("ones_b",), pres], w=[("ps", bl)])
                rl, rlres = rot("of32", of32)
                S.op("dve", (lambda rl=rl, bl=bl: nc.vector.reciprocal(out=rl[:, :], in_=cx.ps[bl][:, :])), r=[("ps", bl)], w=[rlres])
                S.op("dve", (lambda rl=rl, bo=bo: nc.vector.tensor_tensor(out=rl[:, :], in0=cx.ps[bo][:, :], in1=rl[:, :], op=ALU.mult)),
                     r=[("ps", bo), rlres], w=[rlres])
                S.op("sp", (lambda rl=rl, h=h, ts=ts, obT=obT: nc.sync.dma_start(out=obT[h * 128:(h + 1) * 128, ts], in_=rl[:, :])), r=[rlres], w=[("out", "obT")], dma=True)


    cx.end_phase()


def phase_C1(cx, layer, oaT, obT, projT, xT, w_branch, w_out, g_in, w_router, yT, x1T, h2t, comb):
    moe = (layer % 2 == 1)
    nc, S = cx.nc, cx.S
    cx.alloc_wb(2, 512)
    xbuf = cx.sb("xbuf", [128, 32, TT], BF16)
    xst = [cx.sb(f"xst{i}", [128, 2, TT], F32) for i in range(2)]
    xsq = cx.sb("xsq", [128, 2, TT], F32)
    ones_f = cx.sb("ones_f", [128, 128], F32)
    gtile = cx.sb("gtile", [128, 32], F32)
    rstd = cx.sb("rstd", [128, TT], F32)
    ez = [cx.sb(f"ez{i}", [128, NT5], F32) for i in range(2)]
    eo = [cx.sb(f"eo{i}", [128, NT5], F32) for i in range(2)]
    ein = [cx.sb(f"ein{i}", [128, NT5], F32) for i in range(4)]
    eb = [cx.sb(f"eb{i}", [128, NT5], BF16) for i in range(2)]
    pools = {"ez": ez, "eo": eo, "ein": ein, "eb": eb}
    cnt = {k_: 0 for k_ in pools}

    def rot(name):
        pool = pools[name]
        i = cnt[name] % len(pool)
        cnt[name] += 1
        return pool[i], (name, i)

    S.op("pool", lambda: nc.gpsimd.memset(ones_f[:, :], 1.0), w=[("ones",)])
    S.op("sp", lambda: nc.sync.dma_start(out=gtile[:, :], in_=g_in), w=[("gtile",)], dma=True)
    NB = TT // 128
    if moe:
        wr_sb = cx.sb("wr_sb", [128, 32, NEXP], F32)
        ident = cx.sb("ident", [128, 128], F32)
        sel = cx.sb("sel", [8, NEXP, 128], F32)
        lgT = cx.sb("lgT", [8, TT], F32)
        combT = cx.sb("combT", [8, TT], F32)
        cb = cx.sb("cb", [128, TT], F32)
        lg = cx.sb("lg", [128, NB, NEXP], F32)
        mx = cx.sb("mx", [128, NB, 8], F32)
        msk = cx.sb("msk", [128, NB, NEXP], F32)
        den = cx.sb("den", [128, NB], F32)
        xgf = cx.sb("xgf", [128, 2, TT], F32)
        S.op("sp", lambda: nc.sync.dma_start(out=wr_sb[:, :, :], in_=w_router), w=[("wr",)], dma=True)
        S.op("pool", lambda: nc.gpsimd.iota(ident[:, :], pattern=[[1, 128]], base=0, channel_multiplier=-1,
                                            allow_small_or_imprecise_dtypes=True), w=[("ident",)])
        S.op("dve", lambda: nc.vector.tensor_scalar(out=ident[:, :], in0=ident[:, :], scalar1=0.0, scalar2=None, op0=ALU.is_equal),
             r=[("ident",)], w=[("ident",)])
        for e in range(NEXP):
            S.op("dve", (lambda e=e: nc.vector.tensor_copy(out=sel[0:8, e, :], in_=ident[0:8, e:e + 1].to_broadcast([8, 128]))),
                 r=[("ident",)], w=[("sel",)])

    col_blocks = lambda wl, ncols, K, xkeys: globals()['col_blocks'](cx, wl, ncols, K, xkeys)

    tiles_res = lambda name, nrows: [(name, r0, t) for r0 in range(0, nrows, 128) for t in range(TT // NT5)]

    for tt in range(SB // TT):
        tok0 = tt * TT

        def colsl(t, tok0=tok0):
            return slice(tok0 + t * NT5, tok0 + (t + 1) * NT5)

        for half, srcT in enumerate((oaT, obT)):
            srcv = srcT.rearrange("(c p) t -> p c t", p=128)[:, :, tok0:tok0 + TT]
            S.op("pool", (lambda half=half, srcv=srcv: nc.gpsimd.dma_start(out=xbuf[:, half * 16:(half + 1) * 16, :], in_=srcv)),
                 w=[("xb", half)], dma=True)
        jb2 = col_blocks([w_branch[0:HGW, :], w_branch[HGW:D, :]], D, HGW, ["oa", "ob"])

        def epi2(bi, ji, t, accs):
            row0, width = jb2[bi]["meta"][ji]
            cols = colsl(t)
            (pa, ra), (pb, rb) = accs
            ta, rsa = rot("ein")
            tb, rsb = rot("ein")
            S.op("sp", lambda: nc.sync.dma_start(out=ta[0:width, :], in_=projT.rows(R_GA + row0, R_GA + row0 + width)[:, cols]), w=[rsa], dma=True)
            S.op("sp", lambda: nc.sync.dma_start(out=tb[0:width, :], in_=projT.rows(R_GB + row0, R_GB + row0 + width)[:, cols]), w=[rsb], dma=True)
            S.op("dve", lambda: nc.vector.tensor_tensor(out=ta[0:width, :], in0=pa, in1=ta[0:width, :], op=ALU.mult), r=[ra, rsa], w=[rsa])
            S.op("dve", lambda: nc.vector.tensor_tensor(out=tb[0:width, :], in0=pb, in1=tb[0:width, :], op=ALU.mult), r=[rb, rsb], w=[rsb])
            to, rso = rot("eb")
            S.op("dve", lambda: nc.vector.tensor_tensor(out=to[0:width, :], in0=ta[0:width, :], in1=tb[0:width, :], op=ALU.add),
                 r=[rsa, rsb], w=[rso])
            S.op("sp", lambda: nc.sync.dma_start(out=yT[row0:row0 + width, cols], in_=to[0:width, :]), r=[rso], w=[("yT", row0, t)], dma=True)

        gemm(cx, jb2, {"oa": (xbuf[:, 0:16, :], [("xb", 0)], 16), "ob": (xbuf[:, 16:32, :], [("xb", 1)], 16)}, TT, epi2)

        srcv = yT.rearrange("(c p) t -> p c t", p=128)[:, :, tok0:tok0 + TT]
        S.op("sp", (lambda srcv=srcv: nc.sync.dma_start(out=xbuf[:, :, :], in_=srcv)),
             r=tiles_res("yT", D), w=[("xb", 0), ("xb", 1)], dma=True)
        jb3 = col_blocks([w_out], D, D, ["y"])

        def epi3(bi, ji, t, accs):
            row0, width = jb3[bi]["meta"][ji]
            cols = colsl(t)
            (pa, ra), = accs
            ta, rsa = rot("ein")
            S.op("sp", lambda: nc.sync.dma_start(out=ta[0:width, :], in_=xT[row0:row0 + width, cols]), w=[rsa], dma=True)
            S.op("dve", lambda: nc.vector.tensor_tensor(out=ta[0:width, :], in0=pa, in1=ta[0:width, :], op=ALU.add), r=[ra, rsa], w=[rsa])
            S.op("sp", lambda: nc.sync.dma_start(out=x1T[row0:row0 + width, cols], in_=ta[0:width, :]), r=[rsa], w=[("x1T", row0, t)], dma=True)

        gemm(cx, jb3, {"y": (xbuf, [("xb", 0), ("xb", 1)], 32)}, TT, epi3)

        load_x_norm(cx, x1T, tok0, TT, gtile, xbuf, [("xb", 0)], xst, xsq, ones_f, rstd,
                    src_res=tiles_res("x1T", D) + [("xb", 1)],
                    router=(dict(wr=wr_sb, xgf=xgf, lgT=lgT) if moe else None), want_xg=False)

        if moe:
            pi = cx.next_ps()
            for b in range(NB):
                S.op("pe", (lambda b=b: nc.tensor.transpose(out=cx.ps[pi][:, b * 8:(b + 1) * 8], in_=lgT[0:8, b * 128:(b + 1) * 128],
                                                            identity=ident[0:8, 0:8])),
                     r=[("lgT",), ("ident",)], w=[("ps", pi)])
            S.op("dve", lambda: nc.vector.tensor_copy(out=lg[:, :, :], in_=cx.ps[pi][:, 0:NB * 8].rearrange("p (b e) -> p b e", e=8)),
                 r=[("ps", pi)], w=[("lg",)])
            for b in range(NB):
                S.op("dve", (lambda b=b: nc.vector.max(out=mx[:, b, :], in_=lg[:, b, :])), r=[("lg",)], w=[("mx",)])
            S.op("dve", lambda: nc.vector.tensor_tensor(out=msk[:, :, :], in0=lg[:, :, :], in1=mx[:, :, 1:2].to_broadcast([128, NB, NEXP]), op=ALU.is_ge),
                 r=[("lg",), ("mx",)], w=[("msk",)])
            S.op("dve", lambda: nc.vector.tensor_tensor(out=lg[:, :, :], in0=lg[:, :, :], in1=mx[:, :, 0:1].to_broadcast([128, NB, NEXP]), op=ALU.subtract),
                 r=[("lg",), ("mx",), ("msk",)], w=[("lg",)])
            S.op("act", lambda: nc.scalar.activation(out=lg[:, :, :], in_=lg[:, :, :], func=AF.Exp), r=[("lg",)], w=[("lg",)])
            S.op("dve", lambda: nc.vector.tensor_tensor(out=lg[:, :, :], in0=lg[:, :, :], in1=msk[:, :, :], op=ALU.mult), r=[("lg",), ("msk",)], w=[("lg",)])
            S.op("dve", lambda: nc.vector.reduce_sum(out=den[:, :], in_=lg[:, :, :], axis=AX.X), r=[("lg",)], w=[("den",)])
            S.op("dve", lambda: nc.vector.reciprocal(out=den[:, :], in_=den[:, :]), r=[("den",)], w=[("den",)])
            S.op("dve", lambda: nc.vector.tensor_tensor(out=lg[:, :, :], in0=lg[:, :, :], in1=den[:, :].unsqueeze(2).to_broadcast([128, NB, NEXP]), op=ALU.mult),
                 r=[("lg",), ("den",)], w=[("lg",)])
            pjs = [cx.next_ps() for _ in range(TT // NT5)]
            for b in range(NB):
                pp, bb = pjs[b // 4], b % 4
                S.op("pe", (lambda b=b, pp=pp, bb=bb: nc.tensor.transpose(out=cx.ps[pp][0:8, bb * 128:(bb + 1) * 128], in_=lg[:, b, :], identity=ident[:, :])),
                     r=[("lg",), ("ident",)], w=[("ps", pp)])
            for hh, pp in enumerate(pjs):
                S.op("dve", (lambda hh=hh, pp=pp: nc.vector.tensor_copy(out=combT[0:8, hh * NT5:(hh + 1) * NT5], in_=cx.ps[pp][0:8, :])),
                     r=[("ps", pp)], w=[("combT",)])

        x1v = x1T.rearrange("(c p) t -> p c t", p=128)
        for pc in range(16):
            st = xst[pc % 2]
            S.op("sp", (lambda st=st, pc=pc, tok0=tok0: nc.sync.dma_start(out=st[:, :, :], in_=x1v[:, 2 * pc:2 * pc + 2, tok0:tok0 + TT])),
                 r=tiles_res("x1T", D), w=[("xst", pc % 2)], dma=True)
            for c_ in range(2):
                ch = 2 * pc + c_
                eng, E = "dve", nc.vector
                S.op(eng, (lambda st=st, c_=c_, ch=ch, E=E: E.scalar_tensor_tensor(out=xbuf[:, ch, :], in0=st[:, c_, :], scalar=gtile[:, ch:ch + 1],
                                                                                 in1=rstd[:, :], op0=ALU.mult, op1=ALU.mult)),
                     r=[("xst", pc % 2), ("gtile",), ("rstd",)], w=[("xb", 0), ("xb", 1)])
        for half in range(TT // NT5):
            t2 = (tok0 // NT5) + half
            S.op("sp", (lambda half=half, t2=t2: nc.sync.dma_start(out=h2t[t2 * 128:(t2 + 1) * 128, :].rearrange("p (c t) -> p c t", t=NT5),
                                                                   in_=xbuf[:, :, half * NT5:(half + 1) * NT5])),
                 r=[("xb", 0), ("xb", 1)], w=[("h2t", t2)], dma=True)
        if moe:
            S.op("sp", (lambda tok0=tok0: nc.sync.dma_start(out=comb[:, tok0:tok0 + TT], in_=combT[0:8, :])), r=[("combT",)], w=[("comb", tok0)], dma=True)
    cx.end_phase()


def gemm_ws(cx, blocks, xbufs, xload, ntiles, epilogue, pre_tile=None):
    nc, S = cx.nc, cx.S
    nx = 0
    for bi, blk in enumerate(blocks):
        lbufs = []
        for (wap, K) in blk["loads"]:
            wi = cx.next_wb()
            KC = K // 128
            ncols = wap.shape[1]
            dst = cx.wb[wi][:, 0:KC, 0:ncols]
            src = wap.rearrange("(c p) n -> p c n", p=128)
            S.op("pool", (lambda dst=dst, src=src: nc.gpsimd.dma_start(out=dst, in_=src)), w=[("wb", wi)], dma=True)
            lbufs.append((wi, KC))
        KC = blk["KC"]
        for tt in range(ntiles):
            xi = nx % len(xbufs)
            nx += 1
            xb = xbufs[xi]
            src = xload(bi, tt)
            S.op("sp", (lambda xb=xb, src=src, KC=KC: nc.sync.dma_start(out=xb[:, 0:KC, :], in_=src.rearrange("p (c t) -> p c t", t=NT5))),
                 w=[("xw", xi)], dma=True)
            if pre_tile is not None:
                pre_tile(bi, tt)
            for ji, job in enumerate(blk["jobs"]):
                accs = []
                for (li, c0, width) in job:
                    wi, KCw = lbufs[li]
                    pi = cx.next_ps()
                    pst = cx.ps[pi][0:width, :]
                    for k in range(KCw):
                        S.op("pe", (lambda pst=pst, wi=wi, k=k, c0=c0, width=width, xb=xb, KCw=KCw:
                                    nc.tensor.matmul(pst, lhsT=cx.wb[wi][:, k, c0:c0 + width], rhs=xb[:, k, :], start=(k == 0), stop=(k == KCw - 1))),
                             r=[("wb", wi), ("xw", xi)], w=[("ps", pi)])
                    accs.append((pst, ("ps", pi)))
                epilogue(bi, ji, tt, accs)


def ws_blocks(cx, w_ap_list, ncols, K):
    jb = []
    c0 = 0
    while c0 < ncols:
        c1 = min(ncols, c0 + cx.wcols)
        jobs, meta = [], []
        o = 0
        while o < c1 - c0:
            wd = min(128, c1 - c0 - o)
            jobs.append([(li, o, wd) for li in range(len(w_ap_list))])
            meta.append((c0 + o, wd))
            o += wd
        jb.append(dict(loads=[(wap[:, c0:c1], K) for wap in w_ap_list], jobs=jobs, meta=meta, KC=K // 128))
        c0 = c1
    return jb


NT2 = SB // NT5


def phase_F4(cx, experts, h2t, comb, ut):
    nc, S = cx.nc, cx.S
    cx.alloc_wb(3, 512)
    xbufs = [cx.sb(f"xw{i}", [128, 32, NT5], BF16) for i in range(2)]
    eo = [cx.sb(f"eo{i}", [128, NT5], F32) for i in range(2)]
    ez = [cx.sb(f"ez{i}", [128, NT5], F32) for i in range(2)]
    eb = [cx.sb(f"eb{i}", [128, NT5], BF16) for i in range(3)]
    cbt = [cx.sb(f"cb{i}", [128, NT5], F32) for i in range(2)]
    cnt = {"eo": 0, "ez": 0, "eb": 0, "cb": 0}
    pools = {"eo": eo, "ez": ez, "eb": eb, "cb": cbt}

    def rot(name):
        i = cnt[name] % len(pools[name])
        cnt[name] += 1
        return pools[name][i], (name, i)

    for (wa, wb_, FFe, e, ff_base) in experts:
        blocks = ws_blocks(cx, [wa, wb_], FFe, D)
        cur = {}

        def pre_tile(bi, tt, e=e, cur=cur):
            if e is None:
                return
            t_, r_ = rot("cb")
            S.op("sp", (lambda t_=t_, tt=tt: nc.sync.dma_start(out=t_[:, :], in_=comb[e:e + 1, tt * NT5:(tt + 1) * NT5].to_broadcast([128, NT5]))),
                 w=[r_], dma=True)
            cur["cb"] = (t_, r_)

        def epi(bi, ji, tt, accs, blocks=blocks, e=e, ff_base=ff_base, cur=cur):
            row0, width = blocks[bi]["meta"][ji]
            (pa, ra), (pb, rb) = accs
            to, ro = rot("eo")
            S.op("act", lambda: nc.scalar.activation(out=to[0:width, :], in_=pa, func=AF.Silu), r=[ra], w=[ro])
            tb, rb2 = rot("eb")
            if e is not None:
                cbt_, cbr = cur["cb"]
                tz, rz = rot("ez")
                S.op("dve", lambda: nc.vector.tensor_tensor(out=tz[0:width, :], in0=pb, in1=cbt_[0:width, :], op=ALU.mult), r=[rb, cbr], w=[rz])
                S.op("pool", lambda: nc.gpsimd.tensor_tensor(out=tb[0:width, :], in0=to[0:width, :], in1=tz[0:width, :], op=ALU.mult), r=[ro, rz], w=[rb2])
            else:
                S.op("dve", lambda: nc.vector.tensor_tensor(out=tb[0:width, :], in0=pb, in1=to[0:width, :], op=ALU.mult), r=[rb, ro], w=[rb2])
            f = ff_base + row0
            part, ch = f // D, (f % D) // 128
            S.op("sp", lambda: nc.sync.dma_start(out=ut[part][tt * 128:tt * 128 + width, ch * NT5:(ch + 1) * NT5], in_=tb[0:width, :]),
                 r=[rb2], w=[("ut", part, tt, ch)], dma=True)

        gemm_ws(cx, blocks, xbufs, (lambda bi, tt: h2t[tt * 128:(tt + 1) * 128, :]), NT2, epi, pre_tile=pre_tile)
    cx.end_phase()


def phase_F5(cx, parts, ut, x1T, x2T):
    nc, S = cx.nc, cx.S
    cx.alloc_wb(2, 512)
    xbufs = [cx.sb(f"xw{i}", [128, 32, NT5], BF16) for i in range(2)]
    ein = [cx.sb(f"ein{i}", [128, NT5], F32) for i in range(4)]
    cnt = [0]
    for pi_, (w2p, kp) in enumerate(parts):
        blocks = ws_blocks(cx, [w2p], D, kp)
        first = (pi_ == 0)

        def epi(bi, ji, tt, accs, blocks=blocks, first=first):
            row0, width = blocks[bi]["meta"][ji]
            cols = slice(tt * NT5, (tt + 1) * NT5)
            (pa, ra), = accs
            i = cnt[0] % len(ein)
            cnt[0] += 1
            ta, rsa = ein[i], ("ein", i)
            srcT = x1T if first else x2T
            S.op("sp", lambda: nc.sync.dma_start(out=ta[0:width, :], in_=srcT[row0:row0 + width, cols]),
                 r=([] if first else [("x2T", row0, tt)]), w=[rsa], dma=True)
            S.op("dve", lambda: nc.vector.tensor_tensor(out=ta[0:width, :], in0=pa, in1=ta[0:width, :], op=ALU.add), r=[ra, rsa], w=[rsa])
            S.op("sp", lambda: nc.sync.dma_start(out=x2T[row0:row0 + width, cols], in_=ta[0:width, :]), r=[rsa], w=[("x2T", row0, tt)], dma=True)

        gemm_ws(cx, blocks, xbufs, (lambda bi, tt, pi_=pi_, kp=kp: ut[pi_][tt * 128:(tt + 1) * 128, 0:(kp // 128) * NT5]), NT2, epi)
    cx.end_phase()


def phase_FN(cx, x2T, gfin_ap, outT):
    nc, S = cx.nc, cx.S
    xst = [cx.sb(f"xst{i}", [128, 2, TT], F32) for i in range(2)]
    xsq = cx.sb("xsq", [128, 2, TT], F32)
    ones_f = cx.sb("ones_f", [128, 128], F32)
    rstd = cx.sb("rstd", [128, TT], F32)
    gfin = cx.sb("gfin", [128, 32], F32)
    S.op("pool", lambda: nc.gpsimd.memset(ones_f[:, :], 1.0), w=[("ones",)])
    S.op("sp", lambda: nc.sync.dma_start(out=gfin[:, :], in_=gfin_ap), w=[("gfin",)], dma=True)
    x2v = x2T.rearrange("(c p) t -> p c t", p=128)
    outv = outT.rearrange("(c p) t -> p c t", p=128)
    nt = TT // NT5
    for tt in range(SB // TT):
        tok0 = tt * TT
        pss = [cx.next_ps() for _ in range(nt)]
        for pc in range(16):
            st = xst[pc % 2]
            S.op("sp", (lambda st=st, pc=pc, tok0=tok0: nc.sync.dma_start(out=st[:, :, :], in_=x2v[:, 2 * pc:2 * pc + 2, tok0:tok0 + TT])),
                 w=[("xst", pc % 2)], dma=True)
            S.op("act", (lambda st=st: nc.scalar.activation(out=xsq[:, :, :], in_=st[:, :, :], func=AF.Square)), r=[("xst", pc % 2)], w=[("xsq",)])
            for c_ in range(2):
                for t in range(nt):
                    first, last = (pc == 0 and c_ == 0), (pc == 15 and c_ == 1)
                    S.op("pe", (lambda c_=c_, t=t, first=first, last=last, pss=pss: nc.tensor.matmul(cx.ps[pss[t]][:, :], lhsT=ones_f[:, :],
                                                                                                 rhs=xsq[:, c_, t * NT5:(t + 1) * NT5], start=first, stop=last)),
                         r=[("xsq",), ("ones",)], w=[("ps", pss[t])])
        for t in range(nt):
            sl = rstd[:, t * NT5:(t + 1) * NT5]
            S.op("dve", (lambda sl=sl, t=t, pss=pss: nc.vector.tensor_scalar(out=sl, in0=cx.ps[pss[t]][:, :], scalar1=1.0 / D, scalar2=EPS, op0=ALU.mult, op1=ALU.add)),
                 r=[("ps", pss[t])], w=[("rstd",)])
            S.op("act", (lambda sl=sl: nc.scalar.activation(out=sl, in_=sl, func=AF.Sqrt)), r=[("rstd",)], w=[("rstd",)])
            S.op("dve", (lambda sl=sl: nc.vector.reciprocal(out=sl, in_=sl)), r=[("rstd",)], w=[("rstd",)])
        for pc in range(16):
            st = xst[pc % 2]
            S.op("sp", (lambda st=st, pc=pc, tok0=tok0: nc.sync.dma_start(out=st[:, :, :], in_=x2v[:, 2 * pc:2 * pc + 2, tok0:tok0 + TT])),
                 w=[("xst", pc % 2)], dma=True)
            for c_ in range(2):
                ch = 2 * pc + c_
                S.op("dve", (lambda st=st, c_=c_, ch=ch: nc.vector.scalar_tensor_tensor(out=st[:, c_, :], in0=st[:, c_, :], scalar=gfin[:, ch:ch + 1],
                                                                                     in1=rstd[:, :], op0=ALU.mult, op1=ALU.mult)),
                     r=[("xst", pc % 2), ("gfin",), ("rstd",)], w=[("xst", pc % 2)])
            S.op("sp", (lambda st=st, pc=pc, tok0=tok0: nc.sync.dma_start(out=outv[:, 2 * pc:2 * pc + 2, tok0:tok0 + TT], in_=st[:, :, :])),
                 r=[("xst", pc % 2)], w=[("outT",)], dma=True)
    cx.end_phase()


def build_program(depth=2, debug=False):
    cx = Ctx()
    nc = cx.nc
    dbg = set(("oa_tok", "obT", "xA")) if debug else set()

    def din(name, shape, dt=F32):
        return nc.dram_tensor(name, list(shape), dt, kind="ExternalInput").ap()

    def scratch(name, shape, dt=F32):
        return nc.dram_tensor(name, list(shape), dt, kind=("ExternalOutput" if name in dbg else "Internal")).ap()

    xT = din("xT", [D, SB])
    pos = din("pos", [1, SB], I32)
    norm_mix = din("norm_mix", [2, 128, 32])
    w_in = din("w_in", [2, D, IN_COLS])
    lbl = din("lbl", [2, 128, 16])
    hg_norm = din("hg_norm", [2, 1, HGW])
    qg = din("qg", [2, 128, QR // 128])
    kvg = din("kvg", [2, 128, KVR // 128])
    wuq_l = din("wuq_l", [2, QR, 16 * 192])
    wukv_l = din("wukv_l", [2, KVR, 16 * 256])
    w_branch = din("w_branch", [2, D, D])
    w_out = din("w_out", [2, D, D])
    norm_ffn = din("norm_ffn", [2, 128, 32])
    w1 = din("ffn_w1", [D, DFF])
    w3 = din("ffn_w3", [D, DFF])
    w2 = din("ffn_w2", [DFF, D])
    if depth > 1:
        w_router = din("w_router", [128, 32, NEXP])
        mw1 = din("moe_w1", [NEXP, D, D])
        mw3 = din("moe_w3", [NEXP, D, D])
        mw2 = din("moe_w2", [NEXP, D, D])
    gfin = din("norm_final", [128, 32])
    outT = nc.dram_tensor("outT", [D, SB], F32, kind="ExternalOutput").ap()

    projT = RowSplit(nc, "projT", [0, R_HF, R_HI, R_HOG, R_CQ, R_GA, R_GB, IN_COLS, A_ROWS], SB, F32)
    tok = [scratch(f"tok{i}", [SB, HGW]) for i in range(4)]
    oa_tok = scratch("oa_tok", [SB, HGW])
    oaT = scratch("oaT", [HGW, SB])
    obT = scratch("obT", [HGW, SB])
    xA = scratch("xA", [D, SB])
    xB = scratch("xB", [D, SB])
    x1T = scratch("x1T", [D, SB])
    yT = scratch("yT", [D, SB], BF16)
    h2t = scratch("h2t", [NT2 * 128, 32 * NT5], BF16)
    comb = scratch("comb", [NEXP, SB])
    ut = [scratch(f"ut{e}", [NT2 * 128, 32 * NT5], BF16) for e in range(NEXP if depth > 1 else 3)]
    QN = scratch("QN", [HPC, 128, SB], BF16)
    QP = scratch("QP", [HPC, 64, SB], BF16)
    KN = scratch("KN", [HPC, 128, SB], BF16)
    VV = scratch("VV", [SB, HPC * 128], BF16)

    xin = xT
    for l in range(depth):
        last = (l == depth - 1)
        phase_A(cx, l, xin, w_in[l], norm_mix[l], lbl, projT)
        phase_T(cx, [(projT.rows(R_HF, R_HF + HGW), tok[0]), (projT.rows(R_K, R_K + HGW), tok[1]),
                     (projT.rows(R_HI, R_HI + HGW), tok[2]), (projT.rows(R_HOG, R_HOG + HGW), tok[3])])
        phase_Bh(cx, projT, tok[0], tok[1], tok[2], tok[3], hg_norm[l], oa_tok)
        phase_T(cx, [(oa_tok, oaT)])
        phase_Bm(cx, projT.rows(R_CQ, R_CQ + QR), projT.rows(R_CKV, R_CKV + KVR), projT.rows(R_KPE, R_KPE + ROPE), pos, qg[l], kvg[l],
                 wuq_l[l], wukv_l[l], obT, QN, QP, KN, VV)
        xnext = xA if l % 2 == 0 else xB
        moe = (l % 2 == 1)
        phase_C1(cx, l, oaT, obT, projT, xin, w_branch[l], w_out[l], norm_ffn[l], (w_router if moe else None), yT, x1T, h2t, comb)
        if moe:
            experts = [(mw1[e], mw3[e], D, e, e * D) for e in range(NEXP)]
            parts = [(mw2[e], D) for e in range(NEXP)]
        else:
            experts = [(w1, w3, DFF, None, 0)]
            parts = [(w2[r0:min(DFF, r0 + D), :], min(DFF, r0 + D) - r0) for r0 in range(0, DFF, D)]
        phase_F4(cx, experts, h2t, comb, ut)
        phase_F5(cx, parts, ut, x1T, xnext)
        if last:
            phase_FN(cx, xnext, gfin, outT)
        xin = xnext
    st = cx.finish()
    return nc, st


_PROG = {}


def _vec128(v):
    v = np.asarray(v, np.float32)
    return np.ascontiguousarray(v.reshape(-1, 128).T)


def kernel(x, positions, norm_mix, w_in, hg_lb_logits, hg_norm, mla_q_norm, w_uq, mla_kv_norm, w_ukv,
           w_branch, w_out, norm_ffn, ffn_w1, ffn_w3, ffn_w2, w_router, moe_w1, moe_w3, moe_w2, norm_final):
    f = lambda a: np.ascontiguousarray(np.asarray(a, np.float32))
    if "nc" not in _PROG:
        _PROG["nc"], _PROG["st"] = build_program()
    nc = _PROG["nc"]
    x = f(x)
    wuq = f(w_uq).reshape(2, QR, 16, 192)
    wuq_l = np.ascontiguousarray(np.concatenate([wuq[..., :128].reshape(2, QR, 2048), wuq[..., 128:].reshape(2, QR, 1024)], axis=2))
    wukv = f(w_ukv).reshape(2, KVR, 16, 256)
    wukv_l = np.ascontiguousarray(np.concatenate([wukv[..., :128].reshape(2, KVR, 2048), wukv[..., 128:].reshape(2, KVR, 2048)], axis=2))
    shared = {
        "norm_mix": np.stack([_vec128(norm_mix[l]) for l in range(2)]),
        "w_in": f(w_in),
        "lbl": np.stack([_vec128(hg_lb_logits[l]) for l in range(2)]),
        "hg_norm": f(hg_norm).reshape(2, 1, HGW),
        "qg": np.stack([_vec128(mla_q_norm[l]) for l in range(2)]),
        "kvg": np.stack([_vec128(mla_kv_norm[l]) for l in range(2)]),
        "wuq_l": wuq_l, "wukv_l": wukv_l,
        "w_branch": f(w_branch), "w_out": f(w_out),
        "norm_ffn": np.stack([_vec128(norm_ffn[l]) for l in range(2)]),
        "ffn_w1": f(ffn_w1)[0], "ffn_w3": f(ffn_w3)[0], "ffn_w2": f(ffn_w2)[0],
        "w_router": np.ascontiguousarray(f(w_router)[0].reshape(32, 128, NEXP).transpose(1, 0, 2)),
        "moe_w1": f(moe_w1)[0], "moe_w3": f(moe_w3)[0], "moe_w2": f(moe_w2)[0],
        "norm_final": _vec128(norm_final),
    }
    in_maps = []
    for b in range(B_):
        m = dict(shared)
        m["xT"] = np.ascontiguousarray(x[b].T)
        m["pos"] = np.ascontiguousarray(np.asarray(positions, np.int32)[b].reshape(1, SB))
        in_maps.append(m)
    res = run_bass_kernel_spmd(nc, in_maps, core_ids=list(range(NCORES)))
    out = np.stack([np.ascontiguousarray(res.results[b]["outT"].T) for b in range(B_)], axis=0)
    return out.astype(np.float32)
```

```python
import numpy as np
from contextlib import ExitStack
import concourse.bass as bass
import concourse.mybir as mybir
from concourse.bass_utils import run_bass_kernel_spmd

F32 = mybir.dt.float32
BF16 = mybir.dt.bfloat16
I32 = mybir.dt.int32
AF = mybir.ActivationFunctionType
ALU = mybir.AluOpType
AX = mybir.AxisListType

NCORES = 2


class Sched:
    SEM_ROT = 30000
    N_DMA_SEMS = 6

    def __init__(self, nc, es):
        self.nc = nc
        self.es = es
        self.ops = []
        self.engs = {"pe": nc.tensor, "act": nc.scalar, "dve": nc.vector,
                     "pool": nc.gpsimd, "sp": nc.sync}
        self.comp_cnt = {}
        self.dma_cnt = {}
        self.sem_objs = {}
        self.clock = {e: {} for e in self.engs}
        self.tot_ops = 0
        self.tot_waits = 0

    def op(self, eng, fn, r=(), w=(), dma=False, alldma=False):
        self.ops.append((eng, fn, tuple(r), tuple(w), dma, alldma))

    def _newsem(self, name):
        return self.es.enter_context(self.nc.semaphore(name))

    def _sem(self, key):
        s = self.sem_objs.get(key)
        if s is None:
            s = self._newsem("_".join(str(k) for k in key))
            self.sem_objs[key] = s
        return s

    def flush(self):
        ops = self.ops
        n = len(ops)
        last_w = {}
        readers = {}
        deps = [None] * n
        signal = [False] * n
        for i, (eng, fn, r, w, dma, alldma) in enumerate(ops):
            per_eng = {}
            dma_deps = set()
            rset = set(r)

            def add(j, raw):
                oj = ops[j]
                if oj[4]:
                    dma_deps.add(j)
                    return
                if oj[0] == eng and not dma and not raw:
                    return
                if per_eng.get(oj[0], -1) < j:
                    per_eng[oj[0]] = j

            for x in r:
                j = last_w.get(x)
                if j is not None:
                    add(j, True)
            for x in w:
                j = last_w.get(x)
                if j is not None:
                    add(j, x in rset)
                rd = readers.get(x)
                if rd:
                    for e2, j in rd.items():
                        if e2 is None:
                            for jj in j:
                                add(jj, False)
                        else:
                            add(j, False)
            keep = list(per_eng.values()) + list(dma_deps)
            for j in keep:
                signal[j] = True
            deps[i] = keep
            for x in w:
                last_w[x] = i
                readers[x] = {}
            for x in r:
                rd = readers.setdefault(x, {})
                if dma:
                    rd.setdefault(None, []).append(i)
                else:
                    rd[eng] = i

        sig = [None] * n
        guard = [None] * n
        dma_pos = [None] * n
        for i, (eng, fn, r, w, dma, alldma) in enumerate(ops):
            if dma:
                k = self.dma_cnt.get(eng, 0)
                self.dma_cnt[eng] = k + 1
                slot = k % self.N_DMA_SEMS
                key = ("d", eng, slot)
                idx = k // self.N_DMA_SEMS + 1
                sig[i] = (key, 16 * idx)
                if idx > 1:
                    guard[i] = (key, 16 * (idx - 1))
                dma_pos[i] = k + 1
            elif signal[i]:
                k = self.comp_cnt.get(eng, 0)
                self.comp_cnt[eng] = k + 1
                gen = k // self.SEM_ROT
                sig[i] = (("c", eng, gen), k % self.SEM_ROT + 1)

        kc = [None] * n
        issued = dict()
        base_cnt = {e: self.dma_cnt.get(e, 0) - sum(1 for o in ops if o[4] and o[0] == e) for e in self.dma_cnt}
        for e, v in base_cnt.items():
            issued[e] = v
        nwaits = 0
        for i, (eng, fn, r, w, dma, alldma) in enumerate(ops):
            ck = self.clock[eng]
            E = self.engs[eng]
            need = []
            if guard[i] is not None:
                need.append((guard[i], None))
            for j in deps[i]:
                need.append((sig[j], j))
            if alldma:
                for e2, cnt_ in issued.items():
                    for slot in range(min(self.N_DMA_SEMS, cnt_)):
                        nd = (cnt_ - slot + self.N_DMA_SEMS - 1) // self.N_DMA_SEMS
                        need.append(((("d", e2, slot), 16 * nd), None))
            for (key, val), j in need:
                if ck.get(key, 0) >= val:
                    continue
                E.wait_ge(self._sem(key), val)
                nwaits += 1
                ck[key] = val
                if j is not None and kc[j] is not None:
                    for k2, v2 in kc[j].items():
                        if ck.get(k2, 0) < v2:
                            ck[k2] = v2
            inst = fn()
            if dma:
                issued[eng] = dma_pos[i]
            if sig[i] is not None:
                key, val = sig[i]
                inst.then_inc(self._sem(key), 16 if dma else 1)
                snap = dict(ck)
                if snap.get(key, 0) < val:
                    snap[key] = val
                kc[i] = snap
        self.tot_ops += n
        self.tot_waits += nwaits
        self.ops = []

    def stats(self):
        return dict(n_ops=self.tot_ops, n_waits=self.tot_waits, n_sems=len(self.sem_objs))


D = 4096
B_, S_ = 2, 8192
SB = S_
TT = 1024
NT5 = 512
HGW = 2048
NOPE, ROPE, MLA_DV = 128, 64, 128
QR, KVR = 768, 512
IN_COLS = 17728
DFF = 11008
NEXP = 8
EPS = 1e-6
CHUNK = 64
HPC = 4
NHG = 16 // HPC
ATT_SCALE = float((NOPE + ROPE) ** -0.5)
R_HQ, R_HF, R_HI, R_HOG, R_CQ, R_CKV, R_KPE, R_GA, R_GB = 0, 2048, 4096, 6144, 8192, 8960, 9472, 9536, 13632
R_K = IN_COLS
A_ROWS = IN_COLS + 2048


class Ctx:
    def __init__(self):
        self.nc = bass.Bass("TRN2", target_bir_lowering=False)
        self.es = ExitStack()
        self.S = Sched(self.nc, self.es)
        nc = self.nc
        self.ps = [self.es.enter_context(nc.psum_tensor(f"ps{i}", [128, NT5], F32)) for i in range(8)]
        self.ps_i = 0
        self.bar = self.es.enter_context(nc.sbuf_tensor("bar_scr", [128, 8], F32))
        self.pes = ExitStack()
        self.wb = []
        self.wb_i = 0
        self.wcols = 512
        self.nphase = 0
        self.nbar = 0

    def sb(self, name, shape, dt):
        return self.pes.enter_context(self.nc.sbuf_tensor(f"p{self.nphase}_{name}", shape, dt))

    def alloc_wb(self, n, wcols=512):
        self.wcols = wcols
        self.wb = [self.sb(f"wb{i}", [128, 32, wcols], BF16) for i in range(n)]
        self.wb_i = 0

    def next_ps(self):
        i = self.ps_i
        self.ps_i = (i + 1) % 8
        return i

    def next_wb(self):
        i = self.wb_i
        self.wb_i = (i + 1) % len(self.wb)
        return i

    def barrier(self):
        nc, S, bar = self.nc, self.S, self.bar
        k = self.nbar
        self.nbar += 1
        engs = ("pe", "act", "dve", "pool", "sp")

        def tiny(eng, col):
            if eng == "pe":
                return lambda: nc.tensor.matmul(self.ps[7][0:1, 0:1], lhsT=bar[0:1, 6:7], rhs=bar[0:1, 7:8], start=True, stop=True)
            if eng == "act":
                return lambda: nc.scalar.copy(out=bar[0:1, col:col + 1], in_=bar[0:1, 7:8])
            if eng == "dve":
                return lambda: nc.vector.tensor_copy(out=bar[0:1, col:col + 1], in_=bar[0:1, 7:8])
            if eng == "pool":
                return lambda: nc.gpsimd.tensor_copy(out=bar[0:1, col:col + 1], in_=bar[0:1, 7:8])
            return lambda: nc.sync.nop()

        for ci, e in enumerate(engs):
            w = [("barA", k, e)] + ([("ps", 7)] if e == "pe" else [])
            S.op(e, tiny(e, ci), w=w, alldma=(e == "sp"))
        for ci, e in enumerate(engs):
            w = [("barB", k, e)] + ([("ps", 7)] if e == "pe" else [])
            S.op(e, tiny(e, ci), r=[("barA", k, e2) for e2 in engs if e2 != e], w=w)

    def end_phase(self):
        self.barrier()
        self.S.flush()
        self.pes.close()
        self.pes = ExitStack()
        self.wb = []
        self.nphase += 1

    def finish(self):
        self.end_phase()
        st = self.S.stats()
        self.es.close()
        return st


class RowSplit:
    def __init__(self, nc, name, bounds, ncols, dt, kind="Internal"):
        self.bounds = list(bounds)
        self.parts = [nc.dram_tensor(f"{name}_{i}", [bounds[i + 1] - bounds[i], ncols], dt, kind=kind).ap() for i in range(len(bounds) - 1)]

    def rows(self, r0, r1):
        for i in range(len(self.parts)):
            if self.bounds[i] <= r0 and r1 <= self.bounds[i + 1]:
                return self.parts[i][r0 - self.bounds[i]:r1 - self.bounds[i], :]
        raise AssertionError((r0, r1, self.bounds))


def gemm(cx, jobs_blocks, xsrc, T, epilogue):
    nc, S = cx.nc, cx.S
    for bi, blk in enumerate(jobs_blocks):
        lbufs = []
        for (wap, K) in blk["loads"]:
            wi = cx.next_wb()
            KC = K // 128
            ncols = wap.shape[1]
            dst = cx.wb[wi][:, 0:KC, 0:ncols]
            src = wap.rearrange("(c p) n -> p c n", p=128)
            S.op("pool", (lambda dst=dst, src=src: nc.gpsimd.dma_start(out=dst, in_=src)), w=[("wb", wi)], dma=True)
            lbufs.append((wi, KC))
        for ji, job in enumerate(blk["jobs"]):
            for t in range(T // NT5):
                accs = []
                for (li, c0, width, xkey) in job:
                    wi, KC = lbufs[li]
                    xap, xres, xKC = xsrc[xkey]
                    assert xKC == KC
                    pi = cx.next_ps()
                    pst = cx.ps[pi][0:width, :]
                    for k in range(KC):
                        S.op("pe", (lambda pst=pst, wi=wi, k=k, c0=c0, width=width, xap=xap, t=t, KC=KC:
                                    nc.tensor.matmul(pst, lhsT=cx.wb[wi][:, k, c0:c0 + width],
                                                     rhs=xap[:, k, t * NT5:(t + 1) * NT5],
                                                     start=(k == 0), stop=(k == KC - 1))),
                             r=[("wb", wi)] + list(xres), w=[("ps", pi)])
                    accs.append((pst, ("ps", pi)))
                epilogue(bi, ji, t, accs)


def col_blocks(cx, w_ap_list, ncols, K, xkeys):
    jb = []
    c0 = 0
    while c0 < ncols:
        c1 = min(ncols, c0 + cx.wcols)
        jobs, meta = [], []
        o = 0
        while o < c1 - c0:
            wd = min(128, c1 - c0 - o)
            jobs.append([(li, o, wd, xkeys[li]) for li in range(len(w_ap_list))])
            meta.append((c0 + o, wd))
            o += wd
        jb.append(dict(loads=[(wap[:, c0:c1], K) for wap in w_ap_list], jobs=jobs, meta=meta))
        c0 = c1
    return jb


def load_x_norm(cx, xT_ap, tok0, T, gtile, xg, xg_res, xst, xsq, ones_f, rstd, src_res=(), router=None, tag="x", want_xg=True):
    nc, S = cx.nc, cx.S
    KC = D // 128
    PC = 2
    src = xT_ap.rearrange("(c p) t -> p c t", p=128)
    nt = T // NT5
    pss = [cx.next_ps() for _ in range(nt)]
    rps = [cx.next_ps() for _ in range(nt)] if router is not None else None
    npieces = KC // PC
    for pc in range(npieces):
        si = pc % len(xst)
        st = xst[si]
        S.op("sp", (lambda st=st, pc=pc: nc.sync.dma_start(out=st[:, :, 0:T], in_=src[:, pc * PC:(pc + 1) * PC, tok0:tok0 + T])),
             r=list(src_res), w=[(tag + "st", si)], dma=True)
        if router is not None:
            xgf, wr_sb = router["xgf"], router["wr"]
            for c in range(PC):
                ch = pc * PC + c
                S.op("pool", (lambda st=st, c=c, ch=ch: nc.gpsimd.tensor_scalar(out=xgf[:, c, 0:T], in0=st[:, c, 0:T],
                                                                               scalar1=gtile[:, ch:ch + 1], scalar2=None, op0=ALU.mult)),
                     r=[(tag + "st", si), ("gtile",)], w=[("xgf",)])
            for c in range(PC):
                ch = pc * PC + c
                for t in range(nt):
                    first = (pc == 0 and c == 0)
                    last = (pc == npieces - 1 and c == PC - 1)
                    S.op("pe", (lambda t=t, c=c, ch=ch, first=first, last=last:
                                nc.tensor.matmul(cx.ps[rps[t]][0:NEXP, :], lhsT=wr_sb[:, ch, :], rhs=xgf[:, c, t * NT5:(t + 1) * NT5],
                                                 start=first, stop=last)),
                         r=[("xgf",), ("wr",)], w=[("ps", rps[t])])
        S.op("act", (lambda st=st: nc.scalar.activation(out=xsq[:, :, 0:T], in_=st[:, :, 0:T], func=AF.Square)),
             r=[(tag + "st", si)], w=[(tag + "sq",)])
        for c in range(PC):
            for t in range(nt):
                first = (pc == 0 and c == 0)
                last = (pc == npieces - 1 and c == PC - 1)
                S.op("pe", (lambda t=t, c=c, first=first, last=last:
                            nc.tensor.matmul(cx.ps[pss[t]][:, :], lhsT=ones_f[:, :], rhs=xsq[:, c, t * NT5:(t + 1) * NT5],
                                             start=first, stop=last)),
                     r=[(tag + "sq",), ("ones",)], w=[("ps", pss[t])])
        for c in (range(PC) if want_xg else ()):
            ch = pc * PC + c
            S.op("dve", (lambda st=st, c=c, ch=ch: nc.vector.tensor_scalar(out=xg[:, ch, 0:T], in0=st[:, c, 0:T],
                                                                        scalar1=gtile[:, ch:ch + 1], scalar2=None, op0=ALU.mult)),
                 r=[(tag + "st", si), ("gtile",)], w=list(xg_res))
    for t in range(nt):
        sl = rstd[:, t * NT5:(t + 1) * NT5]
        S.op("dve", (lambda sl=sl, t=t: nc.vector.tensor_scalar(out=sl, in0=cx.ps[pss[t]][:, :], scalar1=1.0 / D, scalar2=EPS,
                                                             op0=ALU.mult, op1=ALU.add)),
             r=[("ps", pss[t])], w=[("rstd",)])
        S.op("act", (lambda sl=sl: nc.scalar.activation(out=sl, in_=sl, func=AF.Sqrt)), r=[("rstd",)], w=[("rstd",)])
        S.op("dve", (lambda sl=sl: nc.vector.reciprocal(out=sl, in_=sl)), r=[("rstd",)], w=[("rstd",)])
    if router is not None:
        lgT = router["lgT"]
        for t in range(nt):
            S.op("dve", (lambda t=t: nc.vector.tensor_tensor(out=lgT[0:NEXP, t * NT5:(t + 1) * NT5], in0=cx.ps[rps[t]][0:NEXP, :],
                                                             in1=rstd[0:NEXP, t * NT5:(t + 1) * NT5], op=ALU.mult)),
                 r=[("ps", rps[t]), ("rstd",)], w=[("lgT",)])


def a_subblocks():
    subs = []

    def rng(r0, r1, kind):
        r = r0
        while r < r1:
            wd = min(128, r1 - r)
            subs.append((r, wd, kind))
            r += wd
    rng(R_HQ, R_HF, "silu")
    rng(R_HF, R_HI, "hf")
    rng(R_HI, R_HOG, "id")
    rng(R_HOG, R_CQ, "silu")
    rng(R_CQ, R_GA, "id")
    rng(R_GA, IN_COLS, "sig")
    return subs


def group_blocks(subs, wcols):
    blocks, cur = [], []
    for s in subs:
        if cur and (s[0] + s[1] - cur[0][0] > wcols or s[0] != cur[-1][0] + cur[-1][1]):
            blocks.append(cur)
            cur = []
        cur.append(s)
    if cur:
        blocks.append(cur)
    return blocks


def phase_A(cx, layer, xT, w_in, g_in, lbl, projT):
    nc, S = cx.nc, cx.S
    cx.alloc_wb(3, 512)
    xg = cx.sb("xg", [128, 32, TT], BF16)
    xst = [cx.sb(f"xst{i}", [128, 2, TT], F32) for i in range(2)]
    xsq = cx.sb("xsq", [128, 2, TT], F32)
    ones_f = cx.sb("ones_f", [128, 128], F32)
    gtile = cx.sb("gtile", [128, 32], F32)
    lb = cx.sb("lb", [128, 16], F32)
    oml = cx.sb("oml", [128, 16], F32)
    lbt = cx.sb("lbt", [128, 2, 16], F32)
    rstd = cx.sb("rstd", [128, TT], F32)
    NEZ, NEO = 2, 3
    ez = [cx.sb(f"ez{i}", [128, NT5], F32) for i in range(NEZ)]
    eo = [cx.sb(f"eo{i}", [128, NT5], F32) for i in range(NEO)]
    cnt = {"ez": 0, "eo": 0}

    S.op("pool", lambda: nc.gpsimd.memset(ones_f[:, :], 1.0), w=[("ones",)])
    S.op("sp", lambda: nc.sync.dma_start(out=gtile[:, :], in_=g_in), w=[("gtile",)], dma=True)
    if layer == 0:
        S.op("pool", lambda: nc.gpsimd.memset(lb[:, :], 0.0), w=[("lb",)])
        S.op("pool", lambda: nc.gpsimd.memset(oml[:, :], 1.0), w=[("oml",)])
    else:
        assert layer == 1
        S.op("sp", lambda: nc.sync.dma_start(out=lbt[:, :, :], in_=lbl.rearrange("d p c -> p d c")), w=[("lbt",)], dma=True)
        S.op("dve", lambda: nc.vector.tensor_sub(out=oml[:, :], in0=lbt[:, 1, :], in1=lbt[:, 0, :]), r=[("lbt",)], w=[("oml",)])
        S.op("act", lambda: nc.scalar.activation(out=lb[:, :], in_=oml[:, :], func=AF.Sigmoid), r=[("oml",)], w=[("lb",)])
        S.op("dve", lambda: nc.vector.tensor_scalar(out=oml[:, :], in0=lb[:, :], scalar1=-1.0, scalar2=1.0, op0=ALU.mult, op1=ALU.add),
             r=[("lb",)], w=[("oml",)])

    subs = a_subblocks()
    blocks = group_blocks(subs, cx.wcols)
    jb = []
    for blk in blocks:
        c0 = blk[0][0]
        c1 = blk[-1][0] + blk[-1][1]
        jb.append(dict(loads=[(w_in[:, c0:c1], D)], jobs=[[(0, s[0] - c0, s[1], "x")] for s in blk], meta=blk))

    for tt in range(SB // TT):
        tok0 = tt * TT
        load_x_norm(cx, xT, tok0, TT, gtile, xg, [("xg",)], xst, xsq, ones_f, rstd)

        def epi(bi, ji, t, accs, tok0=tok0):
            row0, width, kind = jb[bi]["meta"][ji]
            pst, pres = accs[0]
            zi = cnt["ez"] % NEZ
            cnt["ez"] += 1
            z = ez[zi][0:width, :]
            rs = rstd[0:width, t * NT5:(t + 1) * NT5]
            S.op("dve", lambda: nc.vector.tensor_tensor(out=z, in0=pst, in1=rs, op=ALU.mult), r=[pres, ("rstd",)], w=[("ez", zi)])
            cols = slice(tok0 + t * NT5, tok0 + (t + 1) * NT5)

            def store(tile_ap, res, r0):
                S.op("sp", lambda: nc.sync.dma_start(out=projT.rows(r0, r0 + width)[:, cols], in_=tile_ap), r=[res], w=[("projT", r0, cols.start)], dma=True)

            def new_eo():
                oi = cnt["eo"] % NEO
                cnt["eo"] += 1
                return oi, eo[oi][0:width, :]

            if kind == "id":
                store(z, ("ez", zi), row0)
            elif kind in ("silu", "sig"):
                oi, o = new_eo()
                fn = AF.Silu if kind == "silu" else AF.Sigmoid
                S.op("act", lambda: nc.scalar.activation(out=o, in_=z, func=fn), r=[("ez", zi)], w=[("eo", oi)])
                store(o, ("eo", oi), row0)
            else:
                ch = (row0 - R_HF) // 128
                oi, sg = new_eo()
                S.op("act", lambda: nc.scalar.activation(out=sg, in_=z, func=AF.Sigmoid), r=[("ez", zi)], w=[("eo", oi)])
                S.op("dve", lambda: nc.vector.tensor_scalar(out=z, in0=sg, scalar1=oml[0:width, ch:ch + 1], scalar2=lb[0:width, ch:ch + 1],
                                                             op0=ALU.mult, op1=ALU.add),
                     r=[("eo", oi), ("oml",), ("lb",)], w=[("ez", zi)])
                oi2, gl = new_eo()
                S.op("act", lambda: nc.scalar.activation(out=gl, in_=z, func=AF.Ln), r=[("ez", zi)], w=[("eo", oi2)])
                store(gl, ("eo", oi2), row0)
                oi3, kk = new_eo()
                S.op("dve", lambda: nc.vector.tensor_scalar(out=kk, in0=z, scalar1=-1.0, scalar2=1.0, op0=ALU.mult, op1=ALU.add),
                     r=[("ez", zi)], w=[("eo", oi3)])
                store(kk, ("eo", oi3), R_K + (row0 - R_HF))

        gemm(cx, jb, {"x": (xg, [("xg",)], 32)}, TT, epi)
    cx.end_phase()


def phase_T(cx, pairs):
    nc, S = cx.nc, cx.S
    ident = cx.sb("ident", [128, 128], F32)
    tin = [cx.sb(f"tin{i}", [128, 4, NT5], F32) for i in range(2)]
    tout = [cx.sb(f"tout{i}", [128, NT5], F32) for i in range(4)]
    S.op("pool", lambda: nc.gpsimd.iota(ident[:, :], pattern=[[1, 128]], base=0, channel_multiplier=-1, allow_small_or_imprecise_dtypes=True),
         w=[("ident",)])
    S.op("dve", lambda: nc.vector.tensor_scalar(out=ident[:, :], in0=ident[:, :], scalar1=0.0, scalar2=None, op0=ALU.is_equal),
         r=[("ident",)], w=[("ident",)])
    it = 0
    no = 0
    for (src, dst) in pairs:
        R, C = src.shape
        for r0 in range(0, R, 512):
            for c0 in range(0, C, 512):
                ti = it % 2
                it += 1
                tl = tin[ti]
                S.op("sp", (lambda tl=tl, src=src, r0=r0, c0=c0: nc.sync.dma_start(
                    out=tl[:, :, :], in_=src[r0:r0 + 512, c0:c0 + 512].rearrange("(j p) c -> p j c", p=128))), w=[("tin", ti)], dma=True)
                for cb in range(4):
                    pi = cx.next_ps()
                    for j in range(4):
                        S.op("pe", (lambda tl=tl, cb=cb, j=j, pi=pi: nc.tensor.transpose(out=cx.ps[pi][:, j * 128:(j + 1) * 128],
                                                                                   in_=tl[:, j, cb * 128:(cb + 1) * 128], identity=ident[:, :])),
                             r=[("tin", ti), ("ident",)], w=[("ps", pi)])
                    oi = no % 4
                    no += 1
                    to = tout[oi]
                    if no % 2 == 0:
                        S.op("dve", (lambda to=to, pi=pi: nc.vector.tensor_copy(out=to[:, :], in_=cx.ps[pi][:, :])), r=[("ps", pi)], w=[("tout", oi)])
                    else:
                        S.op("act", (lambda to=to, pi=pi: nc.scalar.copy(out=to[:, :], in_=cx.ps[pi][:, :])), r=[("ps", pi)], w=[("tout", oi)])
                    S.op("sp", (lambda to=to, dst=dst, cb=cb, r0=r0, c0=c0: nc.sync.dma_start(out=dst[c0 + cb * 128:c0 + (cb + 1) * 128, r0:r0 + 512], in_=to[:, :])),
                         r=[("tout", oi)], w=[("tdst",)], dma=True)
    cx.end_phase()


def phase_Bh(cx, projT, tokG, tokK, tokV, tokOG, gain_all, oa_all):
    nc, S = cx.nc, cx.S
    HW_ = HPC * 128
    SS = 256
    NJ = SS // CHUNK
    NSS = SB // SS
    FW = HPC * SS
    fm = {n: [cx.sb(f"{n}{i}", [128, FW], F32) for i in range(2)] for n in ("q", "g", "k")}
    tm = {n: [cx.sb(f"{n}t{i}", [64, NJ, HW_], F32) for i in range(2)] for n in ("g", "k", "v", "og")}
    bT = cx.sb("bT", [128, FW], F32)
    dd = cx.sb("dd", [128, FW], F32)
    e1 = cx.sb("e1", [128, FW], F32)
    qe = cx.sb("qe", [128, FW], BF16)
    ke = cx.sb("ke", [128, FW], BF16)
    ebm = cx.sb("ebm", [128, HPC * NJ], F32)
    ebl = cx.sb("ebl", [128, HPC * NJ], F32)
    ex = [cx.sb(f"ex{i}", [64, HW_], F32) for i in range(2)]
    kd = cx.sb("kd", [64, NJ, HW_], BF16)
    v_bf = cx.sb("v_bf", [64, NJ, HW_], BF16)
    gg = cx.sb("gg", [64, NJ, HW_], F32)
    o_all = cx.sb("o_all", [64, NJ, HW_], F32)
    sq = cx.sb("sq", [64, NJ, HW_], F32)
    ssum = cx.sb("ssum", [64, NJ * HPC], F32)
    P = cx.sb("P", [64, HPC, CHUNK], BF16)
    St = cx.sb("St", [128, HPC, 128], F32)
    S_bf = cx.sb("S_bf", [128, HPC, 128], BF16)
    segmask = cx.sb("segmask", [128, FW], F32)
    tri = cx.sb("tri", [64, CHUNK], F32)
    U = cx.sb("U", [64, CHUNK], F32)
    gain_bc = cx.sb("gain_bc", [64, HW_], F32)

    S.op("pool", lambda: nc.gpsimd.iota(tri[:, :], pattern=[[1, CHUNK]], base=0, channel_multiplier=-1, allow_small_or_imprecise_dtypes=True), w=[("tri",)])
    S.op("dve", lambda: nc.vector.tensor_scalar(out=U[:, :], in0=tri[:, :], scalar1=0.0, scalar2=None, op0=ALU.is_lt), r=[("tri",)], w=[("U",)])
    S.op("dve", lambda: nc.vector.tensor_scalar(out=tri[:, :], in0=tri[:, :], scalar1=0.0, scalar2=None, op0=ALU.is_ge), r=[("tri",), ("U",)], w=[("tri",)])
    S.op("pool", lambda: nc.gpsimd.iota(segmask[:, :], pattern=[[0, FW // CHUNK], [1, CHUNK]], base=0, channel_multiplier=0, allow_small_or_imprecise_dtypes=True), w=[("seg",)])
    S.op("dve", lambda: nc.vector.tensor_scalar(out=segmask[:, :], in0=segmask[:, :], scalar1=0.0, scalar2=None, op0=ALU.is_gt),
         r=[("seg",)], w=[("seg",)])
    for hg in range(NHG):
        fsl = slice(hg * HW_, (hg + 1) * HW_)
        qT = projT.rows(R_HQ + hg * HW_, R_HQ + (hg + 1) * HW_)
        gT = projT.rows(R_HF + hg * HW_, R_HF + (hg + 1) * HW_)
        kT = projT.rows(R_K + hg * HW_, R_K + (hg + 1) * HW_)
        g_tok, k_tok, v_tok, og_tok = tokG[:, fsl], tokK[:, fsl], tokV[:, fsl], tokOG[:, fsl]
        gain = gain_all[0:1, fsl]
        oa = oa_all[:, fsl]
        S.op("sp", (lambda gain=gain: nc.sync.dma_start(out=gain_bc[:, :], in_=gain.to_broadcast([64, HW_]))), w=[("gain",)], dma=True)
        S.op("pool", lambda: nc.gpsimd.memset(St[:, :, :], 0.0), w=[("S",)])

        PX, PSC, PO, PDS = (0, 1), (2, 3), (4, 5), (6, 7)
        nchunk = 0
        for ss in range(NSS):
            t0 = ss * SS
            bi = ss % 2
            for n, srcT in (("q", qT), ("g", gT), ("k", kT)):
                dst = fm[n][bi]
                S.op("sp", (lambda dst=dst, srcT=srcT, t0=t0: nc.sync.dma_start(out=dst[:, :].rearrange("p (h t) -> p h t", h=HPC),
                                                                                in_=srcT.rearrange("(h c) t -> c h t", c=128)[:, :, t0:t0 + SS])),
                     w=[("fm", n, bi)], dma=True)
            for n, srcT in (("g", g_tok), ("k", k_tok), ("v", v_tok), ("og", og_tok)):
                dst = tm[n][bi]
                S.op("sp", (lambda dst=dst, srcT=srcT, t0=t0: nc.sync.dma_start(out=dst[:, :, :], in_=srcT[t0:t0 + SS, :].rearrange("(j p) f -> p j f", p=64))),
                     w=[("tm", n, bi)], dma=True)
            qs, gs, ks = fm["q"][bi], fm["g"][bi], fm["k"][bi]
            gt, kt, vt, ogt = tm["g"][bi], tm["k"][bi], tm["v"][bi], tm["og"][bi]
            S.op("dve", (lambda gs=gs: nc.vector.tensor_tensor_scan(out=bT[:, :], data0=segmask[:, :], data1=gs[:, :], initial=0.0, op0=ALU.mult, op1=ALU.add)),
                 r=[("fm", "g", bi), ("seg",)], w=[("bT",)])
            bT3 = bT[:, :].rearrange("p (n l) -> p n l", l=CHUNK)
            S.op("dve", lambda: nc.vector.tensor_tensor(out=dd[:, :].rearrange("p (n l) -> p n l", l=CHUNK), in0=bT3,
                                                        in1=bT3[:, :, 31:32].to_broadcast([128, HPC * NJ, CHUNK]), op=ALU.subtract),
                 r=[("bT",)], w=[("dd",)])
            S.op("act", lambda: nc.scalar.activation(out=ebm[:, :], in_=bT3[:, :, 31], func=AF.Exp), r=[("bT",)], w=[("ebm",)])
            S.op("act", lambda: nc.scalar.activation(out=ebl[:, :], in_=bT3[:, :, CHUNK - 1], func=AF.Exp), r=[("bT",)], w=[("ebl",)])
            S.op("act", lambda: nc.scalar.activation(out=e1[:, :], in_=dd[:, :], func=AF.Exp), r=[("dd",)], w=[("e1",)])
            S.op("dve", (lambda qs=qs: nc.vector.tensor_tensor(out=qe[:, :], in0=qs[:, :], in1=e1[:, :], op=ALU.mult)), r=[("fm", "q", bi), ("e1",)], w=[("qe",)])
            S.op("act", lambda: nc.scalar.activation(out=e1[:, :], in_=dd[:, :], func=AF.Exp, scale=-1.0), r=[("dd",), ("qe",)], w=[("e1",)])
            S.op("pool", (lambda ks=ks: nc.gpsimd.tensor_tensor(out=ke[:, :], in0=ks[:, :], in1=e1[:, :], op=ALU.mult)), r=[("fm", "k", bi), ("e1",)], w=[("ke",)])
            S.op("act", (lambda vt=vt: nc.scalar.copy(out=v_bf[:, :, :], in_=vt[:, :, :])), r=[("tm", "v", bi)], w=[("v_bf",)])
            S.op("pool", (lambda ogt=ogt: nc.gpsimd.tensor_tensor(out=gg[:, :, :], in0=ogt[:, :, :], in1=gain_bc[:, :].unsqueeze(1).to_broadcast([64, NJ, HW_]), op=ALU.mult)),
                 r=[("tm", "og", bi), ("gain",)], w=[("gg",)])
            for j in range(NJ):
                px = PX[j % 2]
                S.op("pe", (lambda j=j, px=px, gt=gt: nc.tensor.matmul(cx.ps[px][0:64, :], lhsT=U[:, :], rhs=gt[:, j, :], start=True, stop=True)),
                     r=[("tm", "g", bi), ("U",)], w=[("ps", px)])
                exj = ex[j % 2]
                S.op("act", (lambda px=px, exj=exj: nc.scalar.activation(out=exj[:, :], in_=cx.ps[px][0:64, :], func=AF.Exp)), r=[("ps", px)], w=[("ex", j % 2)])
                S.op("pool", (lambda j=j, exj=exj, kt=kt: nc.gpsimd.tensor_tensor(out=kd[:, j, :], in0=kt[:, j, :], in1=exj[:, :], op=ALU.mult)),
                     r=[("tm", "k", bi), ("ex", j % 2)], w=[("kd", j)])
            for j in range(NJ):
                cs = slice(j * CHUNK, (j + 1) * CHUNK)
                psc, po, pds = PSC[nchunk % 2], PO[nchunk % 2], PDS[nchunk % 2]
                nchunk += 1
                for h in range(HPC):
                    S.op("act", (lambda h=h, j=j: nc.scalar.activation(out=S_bf[:, h, :], in_=St[:, h, :], func=AF.Copy, scale=ebm[:, h * NJ + j:h * NJ + j + 1])),
                         r=[("S",), ("ebm",)], w=[("S_bf",)])
                for h in range(HPC):
                    fs = slice(h * SS + j * CHUNK, h * SS + (j + 1) * CHUNK)
                    S.op("pe", (lambda h=h, fs=fs, psc=psc: nc.tensor.matmul(cx.ps[psc][0:64, h * CHUNK:(h + 1) * CHUNK], lhsT=ke[:, fs], rhs=qe[:, fs], start=True, stop=True)),
                         r=[("ke",), ("qe",)], w=[("ps", psc)])
                S.op("dve", (lambda psc=psc: nc.vector.tensor_tensor(out=P[:, :, :], in0=cx.ps[psc][0:64, 0:HPC * CHUNK].rearrange("p (h t) -> p h t", h=HPC),
                                                                     in1=tri[:, :].unsqueeze(1).to_broadcast([64, HPC, CHUNK]), op=ALU.mult)),
                     r=[("ps", psc), ("tri",)], w=[("P",)])
                for h in range(HPC):
                    fs = slice(h * SS + j * CHUNK, h * SS + (j + 1) * CHUNK)
                    hs = slice(h * 128, (h + 1) * 128)
                    S.op("pe", (lambda h=h, fs=fs, hs=hs, po=po: nc.tensor.matmul(cx.ps[po][0:64, hs], lhsT=qe[:, fs], rhs=S_bf[:, h, :], start=True, stop=False)),
                         r=[("qe",), ("S_bf",)], w=[("ps", po)])
                    S.op("pe", (lambda h=h, j=j, hs=hs, po=po: nc.tensor.matmul(cx.ps[po][0:64, hs], lhsT=P[:, h, :], rhs=v_bf[:, j, hs], start=False, stop=True)),
                         r=[("P",), ("v_bf",)], w=[("ps", po)])
                for h in range(HPC):
                    hs = slice(h * 128, (h + 1) * 128)
                    S.op("pe", (lambda j=j, hs=hs, pds=pds: nc.tensor.matmul(cx.ps[pds][:, hs], lhsT=kd[:, j, hs], rhs=v_bf[:, j, hs], start=True, stop=True)),
                         r=[("kd", j), ("v_bf",)], w=[("ps", pds)])
                for h in range(HPC):
                    hs = slice(h * 128, (h + 1) * 128)
                    S.op("dve", (lambda h=h, j=j, hs=hs, pds=pds: nc.vector.scalar_tensor_tensor(out=St[:, h, :], in0=St[:, h, :], scalar=ebl[:, h * NJ + j:h * NJ + j + 1],
                                                                                              in1=cx.ps[pds][:, hs], op0=ALU.mult, op1=ALU.add)),
                         r=[("S",), ("ebl",), ("ps", pds), ("S_bf",)], w=[("S",)])
                S.op("act", (lambda j=j, po=po: nc.scalar.copy(out=o_all[:, j, :], in_=cx.ps[po][0:64, :])), r=[("ps", po)], w=[("o_all", j)])
            oall_res = [("o_all", j) for j in range(NJ)]
            S.op("pool", lambda: nc.gpsimd.tensor_tensor(out=sq[:, :, :], in0=o_all[:, :, :], in1=o_all[:, :, :], op=ALU.mult), r=oall_res, w=[("sq",)])
            S.op("dve", lambda: nc.vector.reduce_sum(out=ssum[:, :], in_=sq[:, :, :].rearrange("p j (h v) -> p (j h) v", v=128), axis=AX.X), r=[("sq",)], w=[("ssum",)])
            S.op("dve", lambda: nc.vector.tensor_scalar(out=ssum[:, :], in0=ssum[:, :], scalar1=1.0 / 128, scalar2=EPS, op0=ALU.mult, op1=ALU.add), r=[("ssum",)], w=[("ssum",)])
            S.op("act", lambda: nc.scalar.activation(out=ssum[:, :], in_=ssum[:, :], func=AF.Sqrt), r=[("ssum",)], w=[("ssum",)])
            S.op("dve", lambda: nc.vector.reciprocal(out=ssum[:, :], in_=ssum[:, :]), r=[("ssum",)], w=[("ssum",)])
            S.op("dve", lambda: nc.vector.tensor_tensor(out=sq[:, :, :].rearrange("p j (h v) -> p (j h) v", v=128),
                                                        in0=o_all[:, :, :].rearrange("p j (h v) -> p (j h) v", v=128),
                                                        in1=ssum[:, :].unsqueeze(2).to_broadcast([64, NJ * HPC, 128]), op=ALU.mult),
                 r=oall_res + [("ssum",), ("sq",)], w=[("sq",)])
            S.op("pool", lambda: nc.gpsimd.tensor_tensor(out=sq[:, :, :], in0=sq[:, :, :], in1=gg[:, :, :], op=ALU.mult), r=[("sq",), ("gg",)], w=[("sq",)])
            S.op("sp", (lambda t0=t0, oa=oa: nc.sync.dma_start(out=oa[t0:t0 + SS, :].rearrange("(j p) f -> p j f", p=64), in_=sq[:, :, :])),
                 r=[("sq",)], w=[("out", "oa_tok")], dma=True)

    cx.end_phase()


def phase_Bm(cx, cqT, ckvT, kpeT, pos, qg_in, kvg_in, wuq_l, wukv_l, obT_all, QN, QP, KN, VV):
    nc, S = cx.nc, cx.S
    QC, KC = QR // 128, KVR // 128
    wuq_sb = cx.sb("wuq_sb", [128, QC, HPC * 192], BF16)
    wukv_sb = cx.sb("wukv_sb", [128, KC, HPC * 256], BF16)
    qg = cx.sb("qg", [128, QC], F32)
    kvg = cx.sb("kvg", [128, KC], F32)
    ones_f = cx.sb("ones_f", [128, 128], F32)
    ones_b = cx.sb("ones_b", [128, 128], BF16)
    permT = cx.sb("permT", [64, 64], F32)
    invf = cx.sb("invf", [64, 1], F32)
    kp_sb = cx.sb("kp_sb", [64, SB], BF16)
    masks = cx.sb("masks", [128, 4, NT5], BF16)
    cq = cx.sb("cq", [128, QC, NT5], F32)
    cqs = cx.sb("cqs", [128, QC, NT5], F32)
    cqn = cx.sb("cqn", [128, QC, NT5], BF16)
    ckv = cx.sb("ckv", [128, KC, NT5], F32)
    ckvs = cx.sb("ckvs", [128, KC, NT5], F32)
    ckvn = cx.sb("ckvn", [128, KC, NT5], BF16)
    rq = cx.sb("rq", [128, NT5], F32)
    rkv = cx.sb("rkv", [128, NT5], F32)
    rkv_tok = cx.sb("rkv_tok", [128, 4], F32)
    posf = cx.sb("posf", [64, NT5], F32)
    ang = cx.sb("ang", [64, NT5], F32)
    cos2 = cx.sb("cos2", [64, NT5], F32)
    sin2 = cx.sb("sin2", [64, NT5], F32)
    pe_f = [cx.sb(f"pe_f{i}", [64, NT5], F32) for i in range(2)]
    pe_t = [cx.sb(f"pe_t{i}", [64, NT5], F32) for i in range(2)]
    ob16 = [cx.sb(f"ob16_{i}", [128, NT5], BF16) for i in range(3)]
    of32 = [cx.sb(f"of32_{i}", [128, NT5], F32) for i in range(2)]
    cnt = {"pef": 0, "pet": 0, "ob16": 0, "of32": 0}

    def rot(name, pool):
        i = cnt[name] % len(pool)
        cnt[name] += 1
        return pool[i], (name, i)

    S.op("pool", lambda: nc.gpsimd.memset(ones_f[:, :], 1.0), w=[("ones_f",)])
    S.op("pool", lambda: nc.gpsimd.memset(ones_b[:, :], 1.0), w=[("ones_b",)])
    S.op("sp", lambda: nc.sync.dma_start(out=qg[:, :], in_=qg_in), w=[("qg",)], dma=True)
    S.op("sp", lambda: nc.sync.dma_start(out=kvg[:, :], in_=kvg_in), w=[("kvg",)], dma=True)
    S.op("pool", lambda: nc.gpsimd.iota(permT[:, :], pattern=[[1, 64]], base=0, channel_multiplier=-1, allow_small_or_imprecise_dtypes=True),
         w=[("perm",)])
    S.op("dve", lambda: nc.vector.tensor_scalar(out=pe_t[0][:, 0:64], in0=permT[:, :], scalar1=32.0, scalar2=None, op0=ALU.is_equal),
         r=[("perm",)], w=[("perm1",)])
    S.op("dve", lambda: nc.vector.tensor_scalar(out=pe_t[1][:, 0:64], in0=permT[:, :], scalar1=-32.0, scalar2=None, op0=ALU.is_equal),
         r=[("perm",)], w=[("perm2",)])
    S.op("dve", lambda: nc.vector.tensor_sub(out=permT[:, :], in0=pe_t[0][:, 0:64], in1=pe_t[1][:, 0:64]), r=[("perm1",), ("perm2",)], w=[("perm",)])
    S.op("pool", lambda: nc.gpsimd.iota(invf[0:32, :], pattern=[[0, 1]], base=0, channel_multiplier=1, allow_small_or_imprecise_dtypes=True),
         w=[("invf",)])
    S.op("pool", lambda: nc.gpsimd.iota(invf[32:64, :], pattern=[[0, 1]], base=0, channel_multiplier=1, allow_small_or_imprecise_dtypes=True),
         w=[("invf",)])
    S.op("dve", lambda: nc.vector.tensor_scalar(out=invf[:, :], in0=invf[:, :], scalar1=-float(np.log(10000.0) / 32.0), scalar2=None,
                                                 op0=ALU.mult), r=[("invf",)], w=[("invf",)])
    S.op("act", lambda: nc.scalar.activation(out=invf[:, :], in_=invf[:, :], func=AF.Exp), r=[("invf",)], w=[("invf",)])
    S.op("pool", lambda: nc.gpsimd.memset(masks[:, :, :], 0.0), w=[("masks",)])
    for d in range(4):
        S.op("pool", (lambda d=d: nc.gpsimd.memset(masks[0:64, d, 128 * d:NT5], 1.0)), w=[("masks",)])
        if 128 * d + 64 < NT5:
            S.op("pool", (lambda d=d: nc.gpsimd.memset(masks[64:128, d, 128 * d + 64:NT5], 1.0)), w=[("masks",)])

    def ssq_rstd(src, sq, NCH, rt, tag, nfeat):
        S.op("act", lambda: nc.scalar.activation(out=sq[:, :, :], in_=src[:, :, :], func=AF.Square), r=[(tag,)], w=[(tag + "s",)])
        pi = cx.next_ps()
        for c in range(NCH):
            S.op("pe", (lambda c=c: nc.tensor.matmul(cx.ps[pi][:, :], lhsT=ones_f[:, :], rhs=sq[:, c, :], start=(c == 0), stop=(c == NCH - 1))),
                 r=[(tag + "s",), ("ones_f",)], w=[("ps", pi)])
        S.op("dve", lambda: nc.vector.tensor_scalar(out=rt[:, :], in0=cx.ps[pi][:, :], scalar1=1.0 / nfeat, scalar2=EPS, op0=ALU.mult, op1=ALU.add),
             r=[("ps", pi)], w=[(tag + "r",)])
        S.op("act", lambda: nc.scalar.activation(out=rt[:, :], in_=rt[:, :], func=AF.Sqrt), r=[(tag + "r",)], w=[(tag + "r",)])
        S.op("dve", lambda: nc.vector.reciprocal(out=rt[:, :], in_=rt[:, :]), r=[(tag + "r",)], w=[(tag + "r",)])

    def rope(src_f, src_res, dst_ap, dst_res):
        pi = cx.next_ps()
        S.op("pe", lambda: nc.tensor.matmul(cx.ps[pi][0:64, :], lhsT=permT[:, :], rhs=src_f[:, :], start=True, stop=True),
             r=[src_res, ("perm",)], w=[("ps", pi)])
        tmp, tres = rot("pet", pe_t)
        S.op("dve", lambda: nc.vector.tensor_tensor(out=tmp[:, :], in0=cx.ps[pi][0:64, :], in1=sin2[:, :], op=ALU.mult),
             r=[("ps", pi), ("sin2",)], w=[tres])
        S.op("pool", lambda: nc.gpsimd.tensor_tensor(out=src_f[:, :], in0=src_f[:, :], in1=cos2[:, :], op=ALU.mult),
             r=[src_res, ("cos2",)], w=[src_res])
        S.op("dve", lambda: nc.vector.tensor_tensor(out=dst_ap, in0=src_f[:, :], in1=tmp[:, :], op=ALU.add), r=[src_res, tres], w=[dst_res])

    PI = float(np.pi)
    MAGIC = 12582912.0
    C1 = 6.28125
    C2 = float(2.0 * np.pi - 6.28125)
    red = cx.sb("red", [64, NT5], F32)
    red2 = cx.sb("red2", [64, NT5], F32)

    def sincos(dst, dres, shift):
        S.op("dve", lambda: nc.vector.tensor_scalar(out=red2[:, :], in0=ang[:, :], scalar1=shift, scalar2=None, op0=ALU.add), r=[("ang",)], w=[("red2",)])
        S.op("dve", lambda: nc.vector.tensor_scalar(out=red[:, :], in0=red2[:, :], scalar1=1.0 / (2 * PI), scalar2=None, op0=ALU.mult), r=[("red2",)], w=[("red",)])
        S.op("dve", lambda: nc.vector.tensor_scalar(out=red[:, :], in0=red[:, :], scalar1=MAGIC, scalar2=None, op0=ALU.add), r=[("red",)], w=[("red",)])
        S.op("dve", lambda: nc.vector.tensor_scalar(out=red[:, :], in0=red[:, :], scalar1=MAGIC, scalar2=None, op0=ALU.subtract), r=[("red",)], w=[("red",)])
        S.op("dve", lambda: nc.vector.scalar_tensor_tensor(out=red2[:, :], in0=red[:, :], scalar=-C1, in1=red2[:, :], op0=ALU.mult, op1=ALU.add),
             r=[("red",), ("red2",)], w=[("red2",)])
        S.op("dve", lambda: nc.vector.scalar_tensor_tensor(out=red2[:, :], in0=red[:, :], scalar=-C2, in1=red2[:, :], op0=ALU.mult, op1=ALU.add),
             r=[("red",), ("red2",)], w=[("red2",)])
        S.op("dve", lambda: nc.vector.tensor_scalar(out=red2[:, :], in0=red2[:, :], scalar1=PI, scalar2=-PI, op0=ALU.min, op1=ALU.max), r=[("red2",)], w=[("red2",)])
        S.op("act", lambda: nc.scalar.activation(out=dst[:, :], in_=red2[:, :], func=AF.Sin), r=[("red2",)], w=[dres])

    NTL = SB // NT5
    kn_sb = [cx.sb(f"kn{i}", [128, SB], BF16) for i in range(2)]
    v_sb = [cx.sb(f"v{i}", [128, SB // 128, 128], BF16) for i in range(2)]
    qn_sb = [cx.sb(f"qn{i}", [128, NT5], BF16) for i in range(2)]
    qp_sb = [cx.sb(f"qp{i}", [64, NT5], BF16) for i in range(2)]
    pT = [cx.sb(f"pT{i}", [128, NT5], BF16) for i in range(3)]
    npt_box = [0]
    for hg in range(NHG):
        obT = obT_all[hg * HPC * MLA_DV:(hg + 1) * HPC * MLA_DV, :]
        wq_n = wuq_l[:, hg * HPC * 128:(hg + 1) * HPC * 128].rearrange("(c p) n -> p c n", p=128)
        wq_r = wuq_l[:, 16 * 128 + hg * HPC * 64:16 * 128 + (hg + 1) * HPC * 64].rearrange("(c p) n -> p c n", p=128)
        wk_n = wukv_l[:, hg * HPC * 128:(hg + 1) * HPC * 128].rearrange("(c p) n -> p c n", p=128)
        wk_v = wukv_l[:, 16 * 128 + hg * HPC * 128:16 * 128 + (hg + 1) * HPC * 128].rearrange("(c p) n -> p c n", p=128)
        S.op("pool", (lambda wq_n=wq_n: nc.gpsimd.dma_start(out=wuq_sb[:, :, 0:HPC * 128], in_=wq_n)), w=[("wuq",)], dma=True)
        S.op("pool", (lambda wq_r=wq_r: nc.gpsimd.dma_start(out=wuq_sb[:, :, HPC * 128:HPC * 192], in_=wq_r)), w=[("wuq",)], dma=True)
        S.op("pool", (lambda wk_n=wk_n: nc.gpsimd.dma_start(out=wukv_sb[:, :, 0:HPC * 128], in_=wk_n)), w=[("wukv",)], dma=True)
        S.op("pool", (lambda wk_v=wk_v: nc.gpsimd.dma_start(out=wukv_sb[:, :, HPC * 128:HPC * 256], in_=wk_v)), w=[("wukv",)], dma=True)
        for tl in range(SB // NT5):
            t0 = tl * NT5
            ts = slice(t0, t0 + NT5)
            S.op("pool", (lambda ts=ts: nc.gpsimd.dma_start(out=posf[:, :], in_=pos[0:1, ts].to_broadcast([64, NT5]))), w=[("posf",)], dma=True)
            S.op("dve", lambda: nc.vector.tensor_scalar(out=ang[:, :], in0=posf[:, :], scalar1=invf[:, 0:1], scalar2=None, op0=ALU.mult),
                 r=[("posf",), ("invf",)], w=[("ang",)])
            for (dst, dres, shift) in ((sin2, ("sin2",), 0.0), (cos2, ("cos2",), 0.5 * PI)):
                sincos(dst, dres, shift)
            S.op("sp", (lambda ts=ts: nc.sync.dma_start(out=cq[:, :, :], in_=cqT.rearrange("(c p) t -> p c t", p=128)[:, :, ts])), w=[("cq",)], dma=True)
            ssq_rstd(cq, cqs, QC, rq, "cq", QR)
            for c in range(QC):
                S.op("dve", (lambda c=c: nc.vector.tensor_scalar(out=cqn[:, c, :], in0=cq[:, c, :], scalar1=qg[:, c:c + 1], scalar2=None, op0=ALU.mult)),
                     r=[("cq",), ("qg",)], w=[("cqn",)])
            for h in range(HPC):
                pi = cx.next_ps()
                for c in range(QC):
                    S.op("pe", (lambda c=c, h=h, pi=pi: nc.tensor.matmul(cx.ps[pi][:, :], lhsT=wuq_sb[:, c, h * 128:(h + 1) * 128], rhs=cqn[:, c, :],
                                                                       start=(c == 0), stop=(c == QC - 1))),
                         r=[("cqn",), ("wuq",)], w=[("ps", pi)])
                ot, ores = rot("ob16", ob16)
                S.op("dve", (lambda pi=pi, ot=ot: nc.vector.tensor_tensor(out=ot[:, :], in0=cx.ps[pi][:, :], in1=rq[:, :], op=ALU.mult)),
                     r=[("ps", pi), ("cqr",)], w=[ores])
                S.op("sp", (lambda h=h, ot=ot, ts=ts: nc.sync.dma_start(out=QN[h, :, ts], in_=ot[:, :])), r=[ores], w=[("QN", h, tl)], dma=True)
                pj = cx.next_ps()
                for c in range(QC):
                    S.op("pe", (lambda c=c, h=h, pj=pj: nc.tensor.matmul(cx.ps[pj][0:64, :], lhsT=wuq_sb[:, c, HPC * 128 + h * 64:HPC * 128 + (h + 1) * 64],
                                                                       rhs=cqn[:, c, :], start=(c == 0), stop=(c == QC - 1))),
                         r=[("cqn",), ("wuq",)], w=[("ps", pj)])
                pf, pres = rot("pef", pe_f)
                S.op("dve", (lambda pj=pj, pf=pf: nc.vector.tensor_tensor(out=pf[:, :], in0=cx.ps[pj][0:64, :], in1=rq[0:64, :], op=ALU.mult)),
                     r=[("ps", pj), ("cqr",)], w=[pres])
                ot2, ores2 = rot("ob16", ob16)
                rope(pf, pres, ot2[0:64, :], ores2)
                S.op("sp", (lambda h=h, ot2=ot2, ts=ts: nc.sync.dma_start(out=QP[h, :, ts], in_=ot2[0:64, :])), r=[ores2], w=[("QP", h, tl)], dma=True)
            S.op("sp", (lambda ts=ts: nc.sync.dma_start(out=ckv[:, :, :], in_=ckvT.rearrange("(c p) t -> p c t", p=128)[:, :, ts])), w=[("ckv",)], dma=True)
            ssq_rstd(ckv, ckvs, KC, rkv, "ckv", KVR)
            for c in range(KC):
                S.op("dve", (lambda c=c: nc.vector.tensor_scalar(out=ckvn[:, c, :], in0=ckv[:, c, :], scalar1=kvg[:, c:c + 1], scalar2=None, op0=ALU.mult)),
                     r=[("ckv",), ("kvg",)], w=[("ckvn",)])
            for h in range(HPC):
                pi = cx.next_ps()
                for c in range(KC):
                    S.op("pe", (lambda c=c, h=h, pi=pi: nc.tensor.matmul(cx.ps[pi][:, :], lhsT=wukv_sb[:, c, h * 128:(h + 1) * 128], rhs=ckvn[:, c, :],
                                                                       start=(c == 0), stop=(c == KC - 1))),
                         r=[("ckvn",), ("wukv",)], w=[("ps", pi)])
                ot, ores = rot("ob16", ob16)
                S.op("dve", (lambda pi=pi, ot=ot: nc.vector.tensor_tensor(out=ot[:, :], in0=cx.ps[pi][:, :], in1=rkv[:, :], op=ALU.mult)),
                     r=[("ps", pi), ("ckvr",)], w=[ores])
                S.op("sp", (lambda h=h, ot=ot, ts=ts: nc.sync.dma_start(out=KN[h, :, ts], in_=ot[:, :])), r=[ores], w=[("KN", h, tl)], dma=True)
            pk = cx.next_ps()
            for tb in range(4):
                for c in range(KC):
                    S.op("pe", (lambda c=c, tb=tb: nc.tensor.matmul(cx.ps[pk][:, tb:tb + 1], lhsT=ckvs[:, c, tb * 128:(tb + 1) * 128], rhs=ones_f[:, 0:1],
                                                                   start=(c == 0), stop=(c == KC - 1))),
                         r=[("ckvs",), ("ones_f",)], w=[("ps", pk)])
            S.op("dve", lambda: nc.vector.tensor_scalar(out=rkv_tok[:, :], in0=cx.ps[pk][:, 0:4], scalar1=1.0 / KVR, scalar2=EPS, op0=ALU.mult, op1=ALU.add),
                 r=[("ps", pk)], w=[("rkt",)])
            S.op("act", lambda: nc.scalar.activation(out=rkv_tok[:, :], in_=rkv_tok[:, :], func=AF.Sqrt), r=[("rkt",)], w=[("rkt",)])
            S.op("dve", lambda: nc.vector.reciprocal(out=rkv_tok[:, :], in_=rkv_tok[:, :]), r=[("rkt",)], w=[("rkt",)])
            for tb in range(4):
                pi = cx.next_ps()
                for c in range(KC):
                    S.op("pe", (lambda c=c, tb=tb, pi=pi: nc.tensor.matmul(cx.ps[pi][:, :], lhsT=ckvn[:, c, tb * 128:(tb + 1) * 128],
                                                                         rhs=wukv_sb[:, c, HPC * 128:HPC * 256], start=(c == 0), stop=(c == KC - 1))),
                         r=[("ckvn",), ("wukv",)], w=[("ps", pi)])
                ot, ores = rot("ob16", ob16)
                S.op("act", (lambda pi=pi, ot=ot, tb=tb: nc.scalar.activation(out=ot[:, :], in_=cx.ps[pi][:, :], func=AF.Copy, scale=rkv_tok[:, tb:tb + 1])),
                     r=[("ps", pi), ("rkt",)], w=[ores])
                S.op("sp", (lambda ot=ot, tb=tb, t0=t0: nc.sync.dma_start(out=VV[t0 + tb * 128:t0 + (tb + 1) * 128, :], in_=ot[:, :])),
                     r=[ores], w=[("VV", tl)], dma=True)
            pf, pres = rot("pef", pe_f)
            S.op("sp", (lambda pf=pf, ts=ts: nc.sync.dma_start(out=pf[:, :], in_=kpeT[:, ts])), w=[pres], dma=True)
            rope(pf, pres, kp_sb[:, ts], ("kp", tl))
        npt = 0
        nq = 0
        for h in range(HPC):
            hb = h % 2
            S.op("sp", (lambda h=h, hb=hb: nc.sync.dma_start(out=kn_sb[hb][:, :], in_=KN[h, :, :])),
                 r=[("KN", h, tl) for tl in range(NTL)], w=[("kn", hb)], dma=True)
            for half in range(2):
                b0, b1 = half * 32, (half + 1) * 32
                S.op("sp", (lambda h=h, hb=hb, b0=b0, b1=b1: nc.sync.dma_start(
                    out=v_sb[hb][:, b0:b1, :], in_=VV[b0 * 128:b1 * 128, h * 128:(h + 1) * 128].rearrange("(b p) v -> p b v", p=128))),
                     r=[("VV", tl) for tl in range(NTL)], w=[("v", hb, half)], dma=True)
            for i in range(NTL):
                qb = nq % 2
                nq += 1
                ts = slice(i * NT5, (i + 1) * NT5)
                S.op("sp", (lambda h=h, qb=qb, ts=ts: nc.sync.dma_start(out=qn_sb[qb][:, :], in_=QN[h, :, ts])), r=[("QN", h, i)], w=[("qn", qb)], dma=True)
                S.op("sp", (lambda h=h, qb=qb, ts=ts: nc.sync.dma_start(out=qp_sb[qb][:, :], in_=QP[h, :, ts])), r=[("QP", h, i)], w=[("qp", qb)], dma=True)
                bo, bl = 2 + qb, 4 + qb
                nkb = 4 * (i + 1)

                def qk(kb, hb=hb, qb=qb):
                    bs = kb % 2
                    ks = slice(kb * 128, (kb + 1) * 128)
                    S.op("pe", lambda: nc.tensor.matmul(cx.ps[bs][:, :], lhsT=kn_sb[hb][:, ks], rhs=qn_sb[qb][:, :], start=True, stop=False),
                         r=[("kn", hb), ("qn", qb)], w=[("ps", bs)])
                    S.op("pe", lambda: nc.tensor.matmul(cx.ps[bs][:, :], lhsT=kp_sb[0:64, ks], rhs=qp_sb[qb][0:64, :], start=False, stop=True),
                         r=[("kp", kb // 4), ("qp", qb)], w=[("ps", bs)])

                qk(0)
                for kb in range(nkb):
                    if kb + 1 < nkb:
                        qk(kb + 1)
                    bs = kb % 2
                    pt = pT[npt % 3]
                    pres = ("pT", npt % 3)
                    npt += 1
                    S.op("act", (lambda bs=bs, pt=pt: nc.scalar.activation(out=pt[:, :], in_=cx.ps[bs][:, :], func=AF.Exp, scale=ATT_SCALE)),
                         r=[("ps", bs)], w=[pres])
                    if kb >= 4 * i:
                        d = kb - 4 * i
                        S.op("pool", (lambda pt=pt, d=d: nc.gpsimd.tensor_tensor(out=pt[:, :], in0=pt[:, :], in1=masks[:, d, :], op=ALU.mult)),
                             r=[pres, ("masks",)], w=[pres])
                    S.op("pe", (lambda kb=kb, pt=pt, hb=hb, bo=bo, nkb=nkb: nc.tensor.matmul(cx.ps[bo][:, :], lhsT=v_sb[hb][:, kb, :], rhs=pt[:, :],
                                                                                         start=(kb == 0), stop=(kb == nkb - 1))),
                         r=[("v", hb, kb // 32), pres], w=[("ps", bo)])
                    S.op("pe", (lambda kb=kb, pt=pt, bl=bl, nkb=nkb: nc.tensor.matmul(cx.ps[bl][:, :], lhsT=ones_b[:, :], rhs=pt[:, :],
                                                                                  start=(kb == 0), stop=(kb == nkb - 1))),
                         r=[("ones_b",), pres], w=[("ps", bl)])
                rl, rlres = rot("of32", of32)
                S.op("dve", (lambda rl=rl, bl=bl: nc.vector.reciprocal(out=rl[:, :], in_=cx.ps[bl][:, :])), r=[("ps", bl)], w=[rlres])
                S.op("dve", (lambda rl=rl, bo=bo: nc.vector.tensor_tensor(out=rl[:, :], in0=cx.ps[bo][:, :], in1=rl[:, :], op=ALU.mult)),
                     r=[("ps", bo), rlres], w=[rlres])
                S.op("sp", (lambda rl=rl, h=h, ts=ts, obT=obT: nc.sync.dma_start(out=obT[h * 128:(h + 1) * 128, ts], in_=rl[:, :])), r=[rlres], w=[("out", "obT")], dma=True)


    cx.end_phase()


def phase_C1(cx, layer, oaT, obT, projT, xT, w_branch, w_out, g_in, w_router, yT, x1T, h2t, comb):
    moe = (layer % 2 == 1)
    nc, S = cx.nc, cx.S
    cx.alloc_wb(2, 512)
    xbuf = cx.sb("xbuf", [128, 32, TT], BF16)
    xst = [cx.sb(f"xst{i}", [128, 2, TT], F32) for i in range(2)]
    xsq = cx.sb("xsq", [128, 2, TT], F32)
    ones_f = cx.sb("ones_f", [128, 128], F32)
    gtile = cx.sb("gtile", [128, 32], F32)
    rstd = cx.sb("rstd", [128, TT], F32)
    ez = [cx.sb(f"ez{i}", [128, NT5], F32) for i in range(2)]
    eo = [cx.sb(f"eo{i}", [128, NT5], F32) for i in range(2)]
    ein = [cx.sb(f"ein{i}", [128, NT5], F32) for i in range(4)]
    eb = [cx.sb(f"eb{i}", [128, NT5], BF16) for i in range(2)]
    pools = {"ez": ez, "eo": eo, "ein": ein, "eb": eb}
    cnt = {k_: 0 for k_ in pools}

    def rot(name):
        pool = pools[name]
        i = cnt[name] % len(pool)
        cnt[name] += 1
        return pool[i], (name, i)

    S.op("pool", lambda: nc.gpsimd.memset(ones_f[:, :], 1.0), w=[("ones",)])
    S.op("sp", lambda: nc.sync.dma_start(out=gtile[:, :], in_=g_in), w=[("gtile",)], dma=True)
    NB = TT // 128
    if moe:
        wr_sb = cx.sb("wr_sb", [128, 32, NEXP], F32)
        ident = cx.sb("ident", [128, 128], F32)
        sel = cx.sb("sel", [8, NEXP, 128], F32)
        lgT = cx.sb("lgT", [8, TT], F32)
        combT = cx.sb("combT", [8, TT], F32)
        cb = cx.sb("cb", [128, TT], F32)
        lg = cx.sb("lg", [128, NB, NEXP], F32)
        mx = cx.sb("mx", [128, NB, 8], F32)
        msk = cx.sb("msk", [128, NB, NEXP], F32)
        den = cx.sb("den", [128, NB], F32)
        xgf = cx.sb("xgf", [128, 2, TT], F32)
        S.op("sp", lambda: nc.sync.dma_start(out=wr_sb[:, :, :], in_=w_router), w=[("wr",)], dma=True)
        S.op("pool", lambda: nc.gpsimd.iota(ident[:, :], pattern=[[1, 128]], base=0, channel_multiplier=-1,
                                            allow_small_or_imprecise_dtypes=True), w=[("ident",)])
        S.op("dve", lambda: nc.vector.tensor_scalar(out=ident[:, :], in0=ident[:, :], scalar1=0.0, scalar2=None, op0=ALU.is_equal),
             r=[("ident",)], w=[("ident",)])
        for e in range(NEXP):
            S.op("dve", (lambda e=e: nc.vector.tensor_copy(out=sel[0:8, e, :], in_=ident[0:8, e:e + 1].to_broadcast([8, 128]))),
                 r=[("ident",)], w=[("sel",)])

    col_blocks = lambda wl, ncols, K, xkeys: globals()['col_blocks'](cx, wl, ncols, K, xkeys)

    tiles_res = lambda name, nrows: [(name, r0, t) for r0 in range(0, nrows, 128) for t in range(TT // NT5)]

    for tt in range(SB // TT):
        tok0 = tt * TT

        def colsl(t, tok0=tok0):
            return slice(tok0 + t * NT5, tok0 + (t + 1) * NT5)

        for half, srcT in enumerate((oaT, obT)):
            srcv = srcT.rearrange("(c p) t -> p c t", p=128)[:, :, tok0:tok0 + TT]
            S.op("pool", (lambda half=half, srcv=srcv: nc.gpsimd.dma_start(out=xbuf[:, half * 16:(half + 1) * 16, :], in_=srcv)),
                 w=[("xb", half)], dma=True)
        jb2 = col_blocks([w_branch[0:HGW, :], w_branch[HGW:D, :]], D, HGW, ["oa", "ob"])

        def epi2(bi, ji, t, accs):
            row0, width = jb2[bi]["meta"][ji]
            cols = colsl(t)
            (pa, ra), (pb, rb) = accs
            ta, rsa = rot("ein")
            tb, rsb = rot("ein")
            S.op("sp", lambda: nc.sync.dma_start(out=ta[0:width, :], in_=projT.rows(R_GA + row0, R_GA + row0 + width)[:, cols]), w=[rsa], dma=True)
            S.op("sp", lambda: nc.sync.dma_start(out=tb[0:width, :], in_=projT.rows(R_GB + row0, R_GB + row0 + width)[:, cols]), w=[rsb], dma=True)
            S.op("dve", lambda: nc.vector.tensor_tensor(out=ta[0:width, :], in0=pa, in1=ta[0:width, :], op=ALU.mult), r=[ra, rsa], w=[rsa])
            S.op("dve", lambda: nc.vector.tensor_tensor(out=tb[0:width, :], in0=pb, in1=tb[0:width, :], op=ALU.mult), r=[rb, rsb], w=[rsb])
            to, rso = rot("eb")
            S.op("dve", lambda: nc.vector.tensor_tensor(out=to[0:width, :], in0=ta[0:width, :], in1=tb[0:width, :], op=ALU.add),
                 r=[rsa, rsb], w=[rso])
            S.op("sp", lambda: nc.sync.dma_start(out=yT[row0:row0 + width, cols], in_=to[0:width, :]), r=[rso], w=[("yT", row0, t)], dma=True)

        gemm(cx, jb2, {"oa": (xbuf[:, 0:16, :], [("xb", 0)], 16), "ob": (xbuf[:, 16:32, :], [("xb", 1)], 16)}, TT, epi2)

        srcv = yT.rearrange("(c p) t -> p c t", p=128)[:, :, tok0:tok0 + TT]
        S.op("sp", (lambda srcv=srcv: nc.sync.dma_start(out=xbuf[:, :, :], in_=srcv)),
             r=tiles_res("yT", D), w=[("xb", 0), ("xb", 1)], dma=True)
        jb3 = col_blocks([w_out], D, D, ["y"])

        def epi3(bi, ji, t, accs):
            row0, width = jb3[bi]["meta"][ji]
            cols = colsl(t)
            (pa, ra), = accs
            ta, rsa = rot("ein")
            S.op("sp", lambda: nc.sync.dma_start(out=ta[0:width, :], in_=xT[row0:row0 + width, cols]), w=[rsa], dma=True)
            S.op("dve", lambda: nc.vector.tensor_tensor(out=ta[0:width, :], in0=pa, in1=ta[0:width, :], op=ALU.add), r=[ra, rsa], w=[rsa])
            S.op("sp", lambda: nc.sync.dma_start(out=x1T[row0:row0 + width, cols], in_=ta[0:width, :]), r=[rsa], w=[("x1T", row0, t)], dma=True)

        gemm(cx, jb3, {"y": (xbuf, [("xb", 0), ("xb", 1)], 32)}, TT, epi3)

        load_x_norm(cx, x1T, tok0, TT, gtile, xbuf, [("xb", 0)], xst, xsq, ones_f, rstd,
                    src_res=tiles_res("x1T", D) + [("xb", 1)],
                    router=(dict(wr=wr_sb, xgf=xgf, lgT=lgT) if moe else None), want_xg=False)

        if moe:
            pi = cx.next_ps()
            for b in range(NB):
                S.op("pe", (lambda b=b: nc.tensor.transpose(out=cx.ps[pi][:, b * 8:(b + 1) * 8], in_=lgT[0:8, b * 128:(b + 1) * 128],
                                                            identity=ident[0:8, 0:8])),
                     r=[("lgT",), ("ident",)], w=[("ps", pi)])
            S.op("dve", lambda: nc.vector.tensor_copy(out=lg[:, :, :], in_=cx.ps[pi][:, 0:NB * 8].rearrange("p (b e) -> p b e", e=8)),
                 r=[("ps", pi)], w=[("lg",)])
            for b in range(NB):
                S.op("dve", (lambda b=b: nc.vector.max(out=mx[:, b, :], in_=lg[:, b, :])), r=[("lg",)], w=[("mx",)])
            S.op("dve", lambda: nc.vector.tensor_tensor(out=msk[:, :, :], in0=lg[:, :, :], in1=mx[:, :, 1:2].to_broadcast([128, NB, NEXP]), op=ALU.is_ge),
                 r=[("lg",), ("mx",)], w=[("msk",)])
            S.op("dve", lambda: nc.vector.tensor_tensor(out=lg[:, :, :], in0=lg[:, :, :], in1=mx[:, :, 0:1].to_broadcast([128, NB, NEXP]), op=ALU.subtract),
                 r=[("lg",), ("mx",), ("msk",)], w=[("lg",)])
            S.op("act", lambda: nc.scalar.activation(out=lg[:, :, :], in_=lg[:, :, :], func=AF.Exp), r=[("lg",)], w=[("lg",)])
            S.op("dve", lambda: nc.vector.tensor_tensor(out=lg[:, :, :], in0=lg[:, :, :], in1=msk[:, :, :], op=ALU.mult), r=[("lg",), ("msk",)], w=[("lg",)])
            S.op("dve", lambda: nc.vector.reduce_sum(out=den[:, :], in_=lg[:, :, :], axis=AX.X), r=[("lg",)], w=[("den",)])
            S.op("dve", lambda: nc.vector.reciprocal(out=den[:, :], in_=den[:, :]), r=[("den",)], w=[("den",)])
            S.op("dve", lambda: nc.vector.tensor_tensor(out=lg[:, :, :], in0=lg[:, :, :], in1=den[:, :].unsqueeze(2).to_broadcast([128, NB, NEXP]), op=ALU.mult),
                 r=[("lg",), ("den",)], w=[("lg",)])
            pjs = [cx.next_ps() for _ in range(TT // NT5)]
            for b in range(NB):
                pp, bb = pjs[b // 4], b % 4
                S.op("pe", (lambda b=b, pp=pp, bb=bb: nc.tensor.transpose(out=cx.ps[pp][0:8, bb * 128:(bb + 1) * 128], in_=lg[:, b, :], identity=ident[:, :])),
                     r=[("lg",), ("ident",)], w=[("ps", pp)])
            for hh, pp in enumerate(pjs):
                S.op("dve", (lambda hh=hh, pp=pp: nc.vector.tensor_copy(out=combT[0:8, hh * NT5:(hh + 1) * NT5], in_=cx.ps[pp][0:8, :])),
                     r=[("ps", pp)], w=[("combT",)])

        x1v = x1T.rearrange("(c p) t -> p c t", p=128)
        for pc in range(16):
            st = xst[pc % 2]
            S.op("sp", (lambda st=st, pc=pc, tok0=tok0: nc.sync.dma_start(out=st[:, :, :], in_=x1v[:, 2 * pc:2 * pc + 2, tok0:tok0 + TT])),
                 r=tiles_res("x1T", D), w=[("xst", pc % 2)], dma=True)
            for c_ in range(2):
                ch = 2 * pc + c_
                eng, E = "dve", nc.vector
                S.op(eng, (lambda st=st, c_=c_, ch=ch, E=E: E.scalar_tensor_tensor(out=xbuf[:, ch, :], in0=st[:, c_, :], scalar=gtile[:, ch:ch + 1],
                                                                                 in1=rstd[:, :], op0=ALU.mult, op1=ALU.mult)),
                     r=[("xst", pc % 2), ("gtile",), ("rstd",)], w=[("xb", 0), ("xb", 1)])
        for half in range(TT // NT5):
            t2 = (tok0 // NT5) + half
            S.op("sp", (lambda half=half, t2=t2: nc.sync.dma_start(out=h2t[t2 * 128:(t2 + 1) * 128, :].rearrange("p (c t) -> p c t", t=NT5),
                                                                   in_=xbuf[:, :, half * NT5:(half + 1) * NT5])),
                 r=[("xb", 0), ("xb", 1)], w=[("h2t", t2)], dma=True)
        if moe:
            S.op("sp", (lambda tok0=tok0: nc.sync.dma_start(out=comb[:, tok0:tok0 + TT], in_=combT[0:8, :])), r=[("combT",)], w=[("comb", tok0)], dma=True)
    cx.end_phase()


def gemm_ws(cx, blocks, xbufs, xload, ntiles, epilogue, pre_tile=None):
    nc, S = cx.nc, cx.S
    seq = [(bi, tt) for bi in range(len(blocks)) for tt in range(ntiles)]

    def issue_x(idx):
        bi, tt = seq[idx]
        xi = idx % len(xbufs)
        xb = xbufs[xi]
        KC = blocks[bi]["KC"]
        src = xload(bi, tt)
        S.op("sp", (lambda xb=xb, src=src, KC=KC: nc.sync.dma_start(out=xb[:, 0:KC, :], in_=src.rearrange("p (c t) -> p c t", t=NT5))),
             w=[("xw", xi)], dma=True)

    def issue_w(bi, busy):
        lb, deferred = [], []
        for (wap, K) in blocks[bi]["loads"]:
            wi = cx.next_wb()
            KC = K // 128
            ncols = wap.shape[1]
            dst = cx.wb[wi][:, 0:KC, 0:ncols]
            src = wap.rearrange("(c p) n -> p c n", p=128)

            def rec(dst=dst, src=src, wi=wi):
                S.op("pool", (lambda: nc.gpsimd.dma_start(out=dst, in_=src)), w=[("wb", wi)], dma=True)
            if wi in busy:
                deferred.append(rec)
            else:
                rec()
            lb.append((wi, KC))
        return lb, deferred

    issue_x(0)
    nxt, _d = issue_w(0, set())
    idx = 0
    for bi, blk in enumerate(blocks):
        lbufs = nxt
        deferred = []
        if bi + 1 < len(blocks):
            nxt, deferred = issue_w(bi + 1, set(w for w, _ in lbufs))
        for tt in range(ntiles):
            if idx + 1 < len(seq):
                issue_x(idx + 1)
            xi = idx % len(xbufs)
            xb = xbufs[xi]
            idx += 1
            if pre_tile is not None:
                pre_tile(bi, tt)
            for ji, job in enumerate(blk["jobs"]):
                accs = []
                for (li, c0, width) in job:
                    wi, KCw = lbufs[li]
                    pi = cx.next_ps()
                    pst = cx.ps[pi][0:width, :]
                    for k in range(KCw):
                        S.op("pe", (lambda pst=pst, wi=wi, k=k, c0=c0, width=width, xb=xb, KCw=KCw:
                                    nc.tensor.matmul(pst, lhsT=cx.wb[wi][:, k, c0:c0 + width], rhs=xb[:, k, :], start=(k == 0), stop=(k == KCw - 1))),
                             r=[("wb", wi), ("xw", xi)], w=[("ps", pi)])
                    accs.append((pst, ("ps", pi)))
                epilogue(bi, ji, tt, accs)
        for rec in deferred:
            rec()


def ws_blocks(cx, w_ap_list, ncols, K):
    jb = []
    c0 = 0
    while c0 < ncols:
        c1 = min(ncols, c0 + cx.wcols)
        jobs, meta = [], []
        o = 0
        while o < c1 - c0:
            wd = min(128, c1 - c0 - o)
            jobs.append([(li, o, wd) for li in range(len(w_ap_list))])
            meta.append((c0 + o, wd))
            o += wd
        jb.append(dict(loads=[(wap[:, c0:c1], K) for wap in w_ap_list], jobs=jobs, meta=meta, KC=K // 128))
        c0 = c1
    return jb


NT2 = SB // NT5


def phase_F4(cx, experts, h2t, comb, ut):
    nc, S = cx.nc, cx.S
    cx.alloc_wb(3, 512)
    xbufs = [cx.sb(f"xw{i}", [128, 32, NT5], BF16) for i in range(2)]
    eo = [cx.sb(f"eo{i}", [128, NT5], F32) for i in range(2)]
    ez = [cx.sb(f"ez{i}", [128, NT5], F32) for i in range(2)]
    eb = [cx.sb(f"eb{i}", [128, NT5], BF16) for i in range(3)]
    cbt = [cx.sb(f"cb{i}", [128, NT5], F32) for i in range(2)]
    cnt = {"eo": 0, "ez": 0, "eb": 0, "cb": 0}
    pools = {"eo": eo, "ez": ez, "eb": eb, "cb": cbt}

    def rot(name):
        i = cnt[name] % len(pools[name])
        cnt[name] += 1
        return pools[name][i], (name, i)

    for (wa, wb_, FFe, e, ff_base) in experts:
        blocks = ws_blocks(cx, [wa, wb_], FFe, D)
        cur = {}

        def pre_tile(bi, tt, e=e, cur=cur):
            if e is None:
                return
            t_, r_ = rot("cb")
            S.op("sp", (lambda t_=t_, tt=tt: nc.sync.dma_start(out=t_[:, :], in_=comb[e:e + 1, tt * NT5:(tt + 1) * NT5].to_broadcast([128, NT5]))),
                 w=[r_], dma=True)
            cur["cb"] = (t_, r_)

        def epi(bi, ji, tt, accs, blocks=blocks, e=e, ff_base=ff_base, cur=cur):
            row0, width = blocks[bi]["meta"][ji]
            (pa, ra), (pb, rb) = accs
            to, ro = rot("eo")
            S.op("act", lambda: nc.scalar.activation(out=to[0:width, :], in_=pa, func=AF.Silu), r=[ra], w=[ro])
            tb, rb2 = rot("eb")
            if e is not None:
                cbt_, cbr = cur["cb"]
                tz, rz = rot("ez")
                S.op("dve", lambda: nc.vector.tensor_tensor(out=tz[0:width, :], in0=pb, in1=cbt_[0:width, :], op=ALU.mult), r=[rb, cbr], w=[rz])
                S.op("dve", lambda: nc.vector.tensor_tensor(out=tb[0:width, :], in0=to[0:width, :], in1=tz[0:width, :], op=ALU.mult), r=[ro, rz], w=[rb2])
            else:
                S.op("dve", lambda: nc.vector.tensor_tensor(out=tb[0:width, :], in0=pb, in1=to[0:width, :], op=ALU.mult), r=[rb, ro], w=[rb2])
            f = ff_base + row0
            part, ch = f // D, (f % D) // 128
            S.op("sp", lambda: nc.sync.dma_start(out=ut[part][tt * 128:tt * 128 + width, ch * NT5:(ch + 1) * NT5], in_=tb[0:width, :]),
                 r=[rb2], w=[("ut", part, tt, ch)], dma=True)

        gemm_ws(cx, blocks, xbufs, (lambda bi, tt: h2t[tt * 128:(tt + 1) * 128, :]), NT2, epi, pre_tile=pre_tile)
    cx.end_phase()


def phase_F5(cx, parts, ut, x1T, x2T):
    nc, S = cx.nc, cx.S
    cx.alloc_wb(2, 512)
    xbufs = [cx.sb(f"xw{i}", [128, 32, NT5], BF16) for i in range(2)]
    ein = [cx.sb(f"ein{i}", [128, NT5], F32) for i in range(4)]
    cnt = [0]
    for pi_, (w2p, kp) in enumerate(parts):
        blocks = ws_blocks(cx, [w2p], D, kp)
        first = (pi_ == 0)

        def epi(bi, ji, tt, accs, blocks=blocks, first=first):
            row0, width = blocks[bi]["meta"][ji]
            cols = slice(tt * NT5, (tt + 1) * NT5)
            (pa, ra), = accs
            i = cnt[0] % len(ein)
            cnt[0] += 1
            ta, rsa = ein[i], ("ein", i)
            srcT = x1T if first else x2T
            S.op("sp", lambda: nc.sync.dma_start(out=ta[0:width, :], in_=srcT[row0:row0 + width, cols]),
                 r=([] if first else [("x2T", row0, tt)]), w=[rsa], dma=True)
            S.op("dve", lambda: nc.vector.tensor_tensor(out=ta[0:width, :], in0=pa, in1=ta[0:width, :], op=ALU.add), r=[ra, rsa], w=[rsa])
            S.op("sp", lambda: nc.sync.dma_start(out=x2T[row0:row0 + width, cols], in_=ta[0:width, :]), r=[rsa], w=[("x2T", row0, tt)], dma=True)

        gemm_ws(cx, blocks, xbufs, (lambda bi, tt, pi_=pi_, kp=kp: ut[pi_][tt * 128:(tt + 1) * 128, 0:(kp // 128) * NT5]), NT2, epi)
    cx.end_phase()


def phase_FN(cx, x2T, gfin_ap, outT):
    nc, S = cx.nc, cx.S
    xst = [cx.sb(f"xst{i}", [128, 2, TT], F32) for i in range(2)]
    xsq = cx.sb("xsq", [128, 2, TT], F32)
    ones_f = cx.sb("ones_f", [128, 128], F32)
    rstd = cx.sb("rstd", [128, TT], F32)
    gfin = cx.sb("gfin", [128, 32], F32)
    S.op("pool", lambda: nc.gpsimd.memset(ones_f[:, :], 1.0), w=[("ones",)])
    S.op("sp", lambda: nc.sync.dma_start(out=gfin[:, :], in_=gfin_ap), w=[("gfin",)], dma=True)
    x2v = x2T.rearrange("(c p) t -> p c t", p=128)
    outv = outT.rearrange("(c p) t -> p c t", p=128)
    nt = TT // NT5
    for tt in range(SB // TT):
        tok0 = tt * TT
        pss = [cx.next_ps() for _ in range(nt)]
        for pc in range(16):
            st = xst[pc % 2]
            S.op("sp", (lambda st=st, pc=pc, tok0=tok0: nc.sync.dma_start(out=st[:, :, :], in_=x2v[:, 2 * pc:2 * pc + 2, tok0:tok0 + TT])),
                 w=[("xst", pc % 2)], dma=True)
            S.op("act", (lambda st=st: nc.scalar.activation(out=xsq[:, :, :], in_=st[:, :, :], func=AF.Square)), r=[("xst", pc % 2)], w=[("xsq",)])
            for c_ in range(2):
                for t in range(nt):
                    first, last = (pc == 0 and c_ == 0), (pc == 15 and c_ == 1)
                    S.op("pe", (lambda c_=c_, t=t, first=first, last=last, pss=pss: nc.tensor.matmul(cx.ps[pss[t]][:, :], lhsT=ones_f[:, :],
                                                                                                 rhs=xsq[:, c_, t * NT5:(t + 1) * NT5], start=first, stop=last)),
                         r=[("xsq",), ("ones",)], w=[("ps", pss[t])])
        for t in range(nt):
            sl = rstd[:, t * NT5:(t + 1) * NT5]
            S.op("dve", (lambda sl=sl, t=t, pss=pss: nc.vector.tensor_scalar(out=sl, in0=cx.ps[pss[t]][:, :], scalar1=1.0 / D, scalar2=EPS, op0=ALU.mult, op1=ALU.add)),
                 r=[("ps", pss[t])], w=[("rstd",)])
            S.op("act", (lambda sl=sl: nc.scalar.activation(out=sl, in_=sl, func=AF.Sqrt)), r=[("rstd",)], w=[("rstd",)])
            S.op("dve", (lambda sl=sl: nc.vector.reciprocal(out=sl, in_=sl)), r=[("rstd",)], w=[("rstd",)])
        for pc in range(16):
            st = xst[pc % 2]
            S.op("sp", (lambda st=st, pc=pc, tok0=tok0: nc.sync.dma_start(out=st[:, :, :], in_=x2v[:, 2 * pc:2 * pc + 2, tok0:tok0 + TT])),
                 w=[("xst", pc % 2)], dma=True)
            for c_ in range(2):
                ch = 2 * pc + c_
                S.op("dve", (lambda st=st, c_=c_, ch=ch: nc.vector.scalar_tensor_tensor(out=st[:, c_, :], in0=st[:, c_, :], scalar=gfin[:, ch:ch + 1],
                                                                                     in1=rstd[:, :], op0=ALU.mult, op1=ALU.mult)),
                     r=[("xst", pc % 2), ("gfin",), ("rstd",)], w=[("xst", pc % 2)])
            S.op("sp", (lambda st=st, pc=pc, tok0=tok0: nc.sync.dma_start(out=outv[:, 2 * pc:2 * pc + 2, tok0:tok0 + TT], in_=st[:, :, :])),
                 r=[("xst", pc % 2)], w=[("outT",)], dma=True)
    cx.end_phase()


def build_program(depth=2, debug=False):
    cx = Ctx()
    nc = cx.nc
    dbg = set(("oa_tok", "obT", "xA")) if debug else set()

    def din(name, shape, dt=F32):
        return nc.dram_tensor(name, list(shape), dt, kind="ExternalInput").ap()

    def scratch(name, shape, dt=F32):
        return nc.dram_tensor(name, list(shape), dt, kind=("ExternalOutput" if name in dbg else "Internal")).ap()

    xT = din("xT", [D, SB])
    pos = din("pos", [1, SB], I32)
    norm_mix = din("norm_mix", [2, 128, 32])
    w_in = din("w_in", [2, D, IN_COLS])
    lbl = din("lbl", [2, 128, 16])
    hg_norm = din("hg_norm", [2, 1, HGW])
    qg = din("qg", [2, 128, QR // 128])
    kvg = din("kvg", [2, 128, KVR // 128])
    wuq_l = din("wuq_l", [2, QR, 16 * 192])
    wukv_l = din("wukv_l", [2, KVR, 16 * 256])
    w_branch = din("w_branch", [2, D, D])
    w_out = din("w_out", [2, D, D])
    norm_ffn = din("norm_ffn", [2, 128, 32])
    w1 = din("ffn_w1", [D, DFF])
    w3 = din("ffn_w3", [D, DFF])
    w2 = din("ffn_w2", [DFF, D])
    if depth > 1:
        w_router = din("w_router", [128, 32, NEXP])
        mw1 = din("moe_w1", [NEXP, D, D])
        mw3 = din("moe_w3", [NEXP, D, D])
        mw2 = din("moe_w2", [NEXP, D, D])
    gfin = din("norm_final", [128, 32])
    outT = nc.dram_tensor("outT", [D, SB], F32, kind="ExternalOutput").ap()

    projT = RowSplit(nc, "projT", [0, R_HF, R_HI, R_HOG, R_CQ, R_GA, R_GB, IN_COLS, A_ROWS], SB, F32)
    tok = [scratch(f"tok{i}", [SB, HGW]) for i in range(4)]
    oa_tok = scratch("oa_tok", [SB, HGW])
    oaT = scratch("oaT", [HGW, SB])
    obT = scratch("obT", [HGW, SB])
    xA = scratch("xA", [D, SB])
    xB = scratch("xB", [D, SB])
    x1T = scratch("x1T", [D, SB])
    yT = scratch("yT", [D, SB], BF16)
    h2t = scratch("h2t", [NT2 * 128, 32 * NT5], BF16)
    comb = scratch("comb", [NEXP, SB])
    ut = [scratch(f"ut{e}", [NT2 * 128, 32 * NT5], BF16) for e in range(NEXP if depth > 1 else 3)]
    QN = scratch("QN", [HPC, 128, SB], BF16)
    QP = scratch("QP", [HPC, 64, SB], BF16)
    KN = scratch("KN", [HPC, 128, SB], BF16)
    VV = scratch("VV", [SB, HPC * 128], BF16)

    xin = xT
    for l in range(depth):
        last = (l == depth - 1)
        phase_A(cx, l, xin, w_in[l], norm_mix[l], lbl, projT)
        phase_T(cx, [(projT.rows(R_HF, R_HF + HGW), tok[0]), (projT.rows(R_K, R_K + HGW), tok[1]),
                     (projT.rows(R_HI, R_HI + HGW), tok[2]), (projT.rows(R_HOG, R_HOG + HGW), tok[3])])
        phase_Bh(cx, projT, tok[0], tok[1], tok[2], tok[3], hg_norm[l], oa_tok)
        phase_T(cx, [(oa_tok, oaT)])
        phase_Bm(cx, projT.rows(R_CQ, R_CQ + QR), projT.rows(R_CKV, R_CKV + KVR), projT.rows(R_KPE, R_KPE + ROPE), pos, qg[l], kvg[l],
                 wuq_l[l], wukv_l[l], obT, QN, QP, KN, VV)
        xnext = xA if l % 2 == 0 else xB
        moe = (l % 2 == 1)
        phase_C1(cx, l, oaT, obT, projT, xin, w_branch[l], w_out[l], norm_ffn[l], (w_router if moe else None), yT, x1T, h2t, comb)
        if moe:
            experts = [(mw1[e], mw3[e], D, e, e * D) for e in range(NEXP)]
            parts = [(mw2[e], D) for e in range(NEXP)]
        else:
            experts = [(w1, w3, DFF, None, 0)]
            parts = [(w2[r0:min(DFF, r0 + D), :], min(DFF, r0 + D) - r0) for r0 in range(0, DFF, D)]
        phase_F4(cx, experts, h2t, comb, ut)
        phase_F5(cx, parts, ut, x1T, xnext)
        if last:
            phase_FN(cx, xnext, gfin, outT)
        xin = xnext
    st = cx.finish()
    return nc, st


_PROG = {}


def _vec128(v):
    v = np.asarray(v, np.float32)
    return np.ascontiguousarray(v.reshape(-1, 128).T)


def kernel(x, positions, norm_mix, w_in, hg_lb_logits, hg_norm, mla_q_norm, w_uq, mla_kv_norm, w_ukv,
           w_branch, w_out, norm_ffn, ffn_w1, ffn_w3, ffn_w2, w_router, moe_w1, moe_w3, moe_w2, norm_final):
    f = lambda a: np.ascontiguousarray(np.asarray(a, np.float32))
    if "nc" not in _PROG:
        _PROG["nc"], _PROG["st"] = build_program()
    nc = _PROG["nc"]
    x = f(x)
    wuq = f(w_uq).reshape(2, QR, 16, 192)
    wuq_l = np.ascontiguousarray(np.concatenate([wuq[..., :128].reshape(2, QR, 2048), wuq[..., 128:].reshape(2, QR, 1024)], axis=2))
    wukv = f(w_ukv).reshape(2, KVR, 16, 256)
    wukv_l = np.ascontiguousarray(np.concatenate([wukv[..., :128].reshape(2, KVR, 2048), wukv[..., 128:].reshape(2, KVR, 2048)], axis=2))
    shared = {
        "norm_mix": np.stack([_vec128(norm_mix[l]) for l in range(2)]),
        "w_in": f(w_in),
        "lbl": np.stack([_vec128(hg_lb_logits[l]) for l in range(2)]),
        "hg_norm": f(hg_norm).reshape(2, 1, HGW),
        "qg": np.stack([_vec128(mla_q_norm[l]) for l in range(2)]),
        "kvg": np.stack([_vec128(mla_kv_norm[l]) for l in range(2)]),
        "wuq_l": wuq_l, "wukv_l": wukv_l,
        "w_branch": f(w_branch), "w_out": f(w_out),
        "norm_ffn": np.stack([_vec128(norm_ffn[l]) for l in range(2)]),
        "ffn_w1": f(ffn_w1)[0], "ffn_w3": f(ffn_w3)[0], "ffn_w2": f(ffn_w2)[0],
        "w_router": np.ascontiguousarray(f(w_router)[0].reshape(32, 128, NEXP).transpose(1, 0, 2)),
        "moe_w1": f(moe_w1)[0], "moe_w3": f(moe_w3)[0], "moe_w2": f(moe_w2)[0],
        "norm_final": _vec128(norm_final),
    }
    in_maps = []
    for b in range(B_):
        m = dict(shared)
        m["xT"] = np.ascontiguousarray(x[b].T)
        m["pos"] = np.ascontiguousarray(np.asarray(positions, np.int32)[b].reshape(1, SB))
        in_maps.append(m)
    res = run_bass_kernel_spmd(nc, in_maps, core_ids=list(range(NCORES)))
    out = np.stack([np.ascontiguousarray(res.results[b]["outT"].T) for b in range(B_)], axis=0)
    return out.astype(np.float32)
```
